# Optimizing a Trainium2 kernel written in Bass

```python
import math
import jax
import jax.numpy as jnp
from jax import lax
import numpy as np

D_MODEL = 1024
BATCH = 32
SEQ = 2048
DEPTH = 2

GRID_W = 64
CTX_LEN = 256
N_MOD = 6
EPS = 1e-6
NEG_INF = -1e30

D_MIX = D_MODEL
NA_HEAD_DIM = 64
D_NA = D_MIX // 2
NA_HEADS = D_NA // NA_HEAD_DIM
NA_WIN_H = 8
NA_WIN_W = 16
RPB_H = 2 * NA_WIN_H - 1
RPB_W = 2 * NA_WIN_W - 1
D_HY = D_MIX // 4
HY_ORDER = 2
HY_SHORT = 3
HY_EMB = 33
HY_FILT = 64
HY_FAST_DECAY = 0.3
HY_SLOW_DECAY = 1.5
HY_TARGET = 1e-2
D_CF = D_MIX - D_NA - D_HY
CF_KERNEL = 31
D_IN = 3 * D_NA + (HY_ORDER + 1) * D_HY + 2 * D_CF
N_EXPERTS = 16
EC_CAPACITY = 2
EXPERT_FF = 2 * D_MODEL

kernel_name = 'hybrid_na_hyena_conformer_ecmoe_dit'


def rmsnorm(x, g):
    x32 = x.astype(jnp.float32)
    y = x32 * lax.rsqrt(jnp.mean(x32 * x32, axis=-1, keepdims=True) + EPS)
    return (y * g.astype(jnp.float32)).astype(x.dtype)


def modulate(h, shift, scale):
    return h * (1 + scale) + shift


def adaln(cvec, w_mod, b_mod):
    m = jax.nn.silu(cvec) @ w_mod + b_mod
    return jnp.split(m, N_MOD, axis=-1)


def heads(t):
    return t.reshape(t.shape[0], t.shape[1], NA_HEADS, NA_HEAD_DIM)


def depthwise_conv(x, w, b):
    k = w.shape[0]
    pad = (k - 1) // 2
    y = lax.conv_general_dilated(
        x, w.astype(x.dtype)[:, None, :], window_strides=(1,), padding=[(pad, k - 1 - pad)],
        dimension_numbers=('NWC', 'WIO', 'NWC'), feature_group_count=x.shape[-1])
    return y + b.astype(x.dtype)


def neighbourhood_attention(q, k, v, k_ctx, v_ctx, rpb):
    B, S, H, Dh = q.shape
    rows = S // GRID_W
    wh = min(NA_WIN_H, rows)
    nk = wh * GRID_W
    scale = Dh ** -0.5
    qg = q.reshape(B, rows, GRID_W, H, Dh)
    kg = k.reshape(B, rows, GRID_W, H, Dh)
    vg = v.reshape(B, rows, GRID_W, H, Dh)
    col = jnp.arange(GRID_W)
    c0 = jnp.clip(col - NA_WIN_W // 2, 0, GRID_W - NA_WIN_W)
    col_ok = (col[None, :] >= c0[:, None]) & (col[None, :] < c0[:, None] + NA_WIN_W)
    mask = jnp.broadcast_to(col_ok[:, None, :], (GRID_W, wh, GRID_W)).reshape(GRID_W, nk)
    dc = jnp.clip(col[None, :] - col[:, None], 1 - NA_WIN_W, NA_WIN_W - 1) + NA_WIN_W - 1
    rpb32 = rpb.astype(jnp.float32)

    def row_block(r):
        r0 = jnp.clip(r - wh // 2, 0, rows - wh)
        qb = lax.dynamic_index_in_dim(qg, r, axis=1, keepdims=False)
        kb = lax.dynamic_slice_in_dim(kg, r0, wh, axis=1).reshape(B, nk, H, Dh)
        vb = lax.dynamic_slice_in_dim(vg, r0, wh, axis=1).reshape(B, nk, H, Dh)
        dr = r0 + jnp.arange(wh) - r + NA_WIN_H - 1
        bias = rpb32[:, dr[None, :, None], dc[:, None, :]].reshape(H, GRID_W, nk)
        s_nb = jnp.einsum('bqhd,bkhd->bhqk', qb, kb).astype(jnp.float32) * scale + bias
        s_nb = jnp.where(mask, s_nb, NEG_INF)
        s_cx = jnp.einsum('bqhd,bkhd->bhqk', qb, k_ctx).astype(jnp.float32) * scale
        p = jax.nn.softmax(jnp.concatenate([s_nb, s_cx], axis=-1), axis=-1).astype(v.dtype)
        return (jnp.einsum('bhqk,bkhd->bqhd', p[..., :nk], vb)
                + jnp.einsum('bhqk,bkhd->bqhd', p[..., nk:], v_ctx))

    out = lax.map(row_block, jnp.arange(rows))
    return out.transpose(1, 0, 2, 3, 4).reshape(B, S, H * Dh)


def context_attention(q, k, v):
    s = jnp.einsum('bqhd,bkhd->bhqk', q, k).astype(jnp.float32) * (q.shape[-1] ** -0.5)
    p = jax.nn.softmax(s, axis=-1).astype(v.dtype)
    o = jnp.einsum('bhqk,bkhd->bqhd', p, v)
    return o.reshape(o.shape[0], o.shape[1], -1)


def hyena_filter_spectrum(L, w1, b1, freq, w2, b2, w3):
    f32 = jnp.float32
    pos = jnp.arange(L, dtype=f32)[:, None]
    t = pos / max(L - 1, 1)
    bands = (HY_EMB - 1) // 2
    fb = jnp.linspace(1e-4, bands - 1, bands, dtype=f32)[None, :]
    ang = fb * (2.0 * math.pi) * pos / L
    z = jnp.concatenate([t, jnp.cos(ang), -jnp.sin(ang)], axis=-1)
    fr = freq.astype(f32)
    h = jnp.sin(fr * (z @ w1.astype(f32) + b1.astype(f32)))
    h = jnp.sin(fr * (h @ w2.astype(f32) + b2.astype(f32)))
    h = (h @ w3.astype(f32)).reshape(L, 2, HY_ORDER, D_HY)
    min_decay = math.log(HY_TARGET) / HY_SLOW_DECAY
    max_decay = math.log(HY_TARGET) / HY_FAST_DECAY
    rate = jnp.abs(jnp.linspace(min_decay, max_decay, D_HY, dtype=f32))
    h = h * jnp.exp(-t * rate)[:, None, None, :]
    fwd, bwd = h[:, 0], h[:, 1]
    k_circ = jnp.concatenate([fwd, jnp.zeros_like(fwd[:1]), bwd[1:][::-1]], axis=0)
    return jnp.fft.rfft(k_circ, axis=0)


def hyena_mixer(u, short_w, short_b, bias_d, w1, b1, freq, w2, b2, w3):
    dt = u.dtype
    L = u.shape[1]
    u = depthwise_conv(u, short_w, short_b).astype(jnp.float32)
    x1, x2, v = jnp.split(u, 3, axis=-1)
    k_spec = hyena_filter_spectrum(L, w1, b1, freq, w2, b2, w3)
    d = bias_d.astype(jnp.float32)
    z = v
    for n, gate in enumerate((x1, x2)):
        z_f = jnp.fft.rfft(z, n=2 * L, axis=1)
        conv = jnp.fft.irfft(z_f * k_spec[None, :, n], n=2 * L, axis=1)[:, :L]
        z = gate * (conv + d[n] * z)
    return z.astype(dt)


def conformer_conv(u, dw_w, dw_b, ln_g, ln_b):
    a, g = jnp.split(u, 2, axis=-1)
    y = depthwise_conv(a * jax.nn.sigmoid(g), dw_w, dw_b).astype(jnp.float32)
    mu = jnp.mean(y, axis=-1, keepdims=True)
    var = jnp.mean(jnp.square(y - mu), axis=-1, keepdims=True)
    y = (y - mu) * lax.rsqrt(var + EPS) * ln_g.astype(jnp.float32) + ln_b.astype(jnp.float32)
    return jax.nn.silu(y).astype(u.dtype)


def expert_choice_ffn(h, w_router, w1, w3, w2):
    B, L, _ = h.shape
    cap = EC_CAPACITY * L // N_EXPERTS
    aff = jax.nn.softmax((h @ w_router).astype(jnp.float32), axis=-1)
    gate, idx = lax.top_k(jnp.swapaxes(aff, 1, 2), cap)
    b_idx = jnp.arange(B)[:, None, None]
    xe = h[b_idx, idx]
    a = jnp.einsum('becd,edf->becf', xe, w1)
    u = jnp.einsum('becd,edf->becf', xe, w3)
    y = jnp.einsum('becf,efd->becd', jax.nn.silu(a) * u, w2)
    y = y * gate[..., None].astype(y.dtype)
    return jnp.zeros_like(h).at[b_idx, idx].add(y)


def setup_inputs(seed: int = 0) -> dict:
    key = jax.random.key(seed)
    ks = jax.random.split(key, 32)
    f32 = jnp.float32

    def nrm(k, shape, scale):
        return scale * jax.random.normal(k, shape, f32)

    L = DEPTH
    return {
        'x': nrm(ks[0], (BATCH, SEQ, D_MODEL), 1.0),
        'c': nrm(ks[1], (BATCH, D_MODEL), 1.0),
        'ctx': nrm(ks[2], (BATCH, CTX_LEN, D_MODEL), 1.0),
        'c_ctx': nrm(ks[3], (D_MODEL,), 1.0),
        'w_mod': nrm(ks[4], (L, D_MODEL, N_MOD * D_MODEL), 0.5 * D_MODEL ** -0.5),
        'b_mod': nrm(ks[5], (L, N_MOD * D_MODEL), 0.02),
        'norm1_g': 1.0 + nrm(ks[6], (L, D_MODEL), 0.02),
        'norm2_g': 1.0 + nrm(ks[7], (L, D_MODEL), 0.02),
        'w_in': nrm(ks[8], (L, D_MODEL, D_IN), D_MODEL ** -0.5),
        'na_rpb': nrm(ks[9], (L, NA_HEADS, RPB_H, RPB_W), 0.1),
        'hy_short_w': nrm(ks[10], (L, HY_SHORT, (HY_ORDER + 1) * D_HY), HY_SHORT ** -0.5),
        'hy_short_b': nrm(ks[11], (L, (HY_ORDER + 1) * D_HY), 0.02),
        'hy_filt_w1': nrm(ks[12], (L, HY_EMB, HY_FILT), HY_EMB ** -0.5),
        'hy_filt_b1': nrm(ks[13], (L, HY_FILT), 0.02),
        'hy_filt_freq': 1.0 + nrm(ks[14], (L, HY_FILT), 0.02),
        'hy_filt_w2': nrm(ks[15], (L, HY_FILT, HY_FILT), HY_FILT ** -0.5),
        'hy_filt_b2': nrm(ks[16], (L, HY_FILT), 0.02),
        'hy_filt_w3': nrm(ks[17], (L, HY_FILT, 2 * HY_ORDER * D_HY), 0.05 * HY_FILT ** -0.5),
        'hy_bias_d': nrm(ks[18], (L, HY_ORDER, D_HY), 0.5),
        'cf_dw_w': nrm(ks[19], (L, CF_KERNEL, D_CF), CF_KERNEL ** -0.5),
        'cf_dw_b': nrm(ks[20], (L, D_CF), 0.02),
        'cf_ln_g': 1.0 + nrm(ks[21], (L, D_CF), 0.02),
        'cf_ln_b': nrm(ks[22], (L, D_CF), 0.02),
        'w_out': nrm(ks[23], (L, D_MIX, D_MODEL), D_MIX ** -0.5),
        'router_w': nrm(ks[24], (L, D_MODEL, N_EXPERTS), D_MODEL ** -0.5),
        'expert_w1': nrm(ks[25], (L, N_EXPERTS, D_MODEL, EXPERT_FF), D_MODEL ** -0.5),
        'expert_w3': nrm(ks[26], (L, N_EXPERTS, D_MODEL, EXPERT_FF), D_MODEL ** -0.5),
        'expert_w2': nrm(ks[27], (L, N_EXPERTS, EXPERT_FF, D_MODEL), EXPERT_FF ** -0.5),
        'final_norm_g': 1.0 + nrm(ks[28], (D_MODEL,), 0.02),
    }


def reference(x, c, ctx, c_ctx, w_mod, b_mod, norm1_g, norm2_g, w_in, na_rpb,
              hy_short_w, hy_short_b, hy_filt_w1, hy_filt_b1, hy_filt_freq, hy_filt_w2,
              hy_filt_b2, hy_filt_w3, hy_bias_d, cf_dw_w, cf_dw_b, cf_ln_g, cf_ln_b,
              w_out, router_w, expert_w1, expert_w3, expert_w2, final_norm_g):
    splits = [D_NA, 2 * D_NA, 3 * D_NA, 3 * D_NA + (HY_ORDER + 1) * D_HY]
    xc = ctx
    for l in range(DEPTH):
        last = l == DEPTH - 1
        sh1, sc1, g1, sh2, sc2, g2 = [m[:, None, :] for m in adaln(c, w_mod[l], b_mod[l])]
        csh1, csc1, cg1, csh2, csc2, cg2 = adaln(c_ctx, w_mod[l], b_mod[l])

        def head_groups(attn, hy_in, cf_in):
            hy = hyena_mixer(hy_in, hy_short_w[l], hy_short_b[l], hy_bias_d[l], hy_filt_w1[l], hy_filt_b1[l],
                             hy_filt_freq[l], hy_filt_w2[l], hy_filt_b2[l], hy_filt_w3[l])
            cf = conformer_conv(cf_in, cf_dw_w[l], cf_dw_b[l], cf_ln_g[l], cf_ln_b[l])
            return jnp.concatenate([attn, hy, cf], axis=-1) @ w_out[l]

        h = modulate(rmsnorm(x, norm1_g[l]), sh1, sc1)
        q, k, v, hy_in, cf_in = jnp.split(h @ w_in[l], splits, axis=-1)
        hc = modulate(rmsnorm(xc, norm1_g[l]), csh1, csc1)
        if last:
            kc, vc = jnp.split(hc @ w_in[l][:, D_NA:3 * D_NA], 2, axis=-1)
        else:
            qc, kc, vc, hyc_in, cfc_in = jnp.split(hc @ w_in[l], splits, axis=-1)
        attn = neighbourhood_attention(heads(q), heads(k), heads(v), heads(kc), heads(vc), na_rpb[l])
        x = x + g1 * head_groups(attn, hy_in, cf_in)
        h2 = modulate(rmsnorm(x, norm2_g[l]), sh2, sc2)
        x = x + g2 * expert_choice_ffn(h2, router_w[l], expert_w1[l], expert_w3[l], expert_w2[l])

        if not last:
            attn_c = context_attention(heads(qc), heads(kc), heads(vc))
            xc = xc + cg1 * head_groups(attn_c, hyc_in, cfc_in)
            hc2 = modulate(rmsnorm(xc, norm2_g[l]), csh2, csc2)
            xc = xc + cg2 * expert_choice_ffn(hc2, router_w[l], expert_w1[l], expert_w3[l], expert_w2[l])
    return rmsnorm(x, final_norm_g)
```

```python
import numpy as np
from contextlib import ExitStack
import concourse.bass as bass
import concourse.mybir as mybir

F32 = mybir.dt.float32
BF16 = mybir.dt.bfloat16
U32 = mybir.dt.uint32
I32 = mybir.dt.int32
AF = mybir.ActivationFunctionType
ALU = mybir.AluOpType
AX = mybir.AxisListType

ENGS = ['pe', 'act', 'dve', 'pool', 'sp']
STRICT = {'pe': False, 'act': True, 'dve': True, 'pool': True, 'sp': False}
NDMASEM = 12


class _Call:
    def __init__(self, name, args, kwargs):
        self.name = name; self.args = args; self.kwargs = kwargs
    def __call__(self, engine):
        return getattr(engine, self.name)(*self.args, **self.kwargs)


class _Proxy:
    def __getattr__(self, name):
        def rec(*args, **kwargs):
            return _Call(name, args, kwargs)
        return rec

_PROXY = _Proxy()


class Prog:
    def __init__(self, nc):
        self.nc = nc
        self.es = ExitStack()
        self.streams = {e: [] for e in ENGS}
        self.cnt = {}
        self.seen = {e: {} for e in ENGS}
        self.lastw = {}
        self.readers = {}
        self.sems = {}
        for e in ENGS:
            self.sems[e] = self.es.enter_context(nc.semaphore("s_" + e))
            self.cnt[e] = 0
        self.dq_n = {}
        for q in ['sp', 'pool', 'act']:
            self.dq_n[q] = 0
            for i in range(NDMASEM):
                k = ('dq', q, i)
                self.sems[k] = self.es.enter_context(nc.semaphore("d_%s%d" % (q, i)))
                self.cnt[k] = 0
        self.nops = 0
        self.marked = {e: set() for e in ENGS}

    def sbuf(self, name, shape, dtype):
        return self.es.enter_context(self.nc.sbuf_tensor(name, list(shape), dtype))

    def psum(self, name, shape, dtype=F32):
        return self.es.enter_context(self.nc.psum_tensor(name, list(shape), dtype))

    def _deps(self, eng, reads, writes):
        need = {}
        def add(tok, raw):
            k, v = tok
            if k == eng and not raw:
                return
            if need.get(k, 0) < v:
                need[k] = v
        for b in reads:
            if b in self.lastw:
                add(self.lastw[b], True)
        for b in writes:
            if b in self.lastw:
                add(self.lastw[b], False)
            for t in self.readers.get(b, ()):
                add(t, False)
        st = self.streams[eng]
        for k, v in need.items():
            if k == eng and not STRICT[eng]:
                continue
            if self.seen[eng].get(k, 0) >= v:
                continue
            st.append(('wait', k, v))
            self.seen[eng][k] = v
            if k in self.marked:
                self.marked[k].add(v)

    def _commit(self, tok, reads, writes):
        for b in writes:
            self.lastw[b] = tok
            self.readers[b] = []
        for b in reads:
            self.readers.setdefault(b, []).append(tok)

    def op(self, eng, fn, reads=(), writes=()):
        reads = list(reads); writes = list(writes)
        self._deps(eng, reads, writes)
        self.cnt[eng] += 1
        fn = fn(_PROXY)
        assert isinstance(fn, _Call)
        self.streams[eng].append(('op', fn, eng, self.cnt[eng], getattr(self, 'stage', '')))
        self._commit((eng, self.cnt[eng]), reads, writes)
        self.nops += 1

    def dma(self, q, out, in_, reads=(), writes=(), fn=None, **kw):
        reads = list(reads); writes = list(writes)
        self._deps(q, reads, writes)
        i = self.dq_n[q] % NDMASEM
        self.dq_n[q] += 1
        k = ('dq', q, i)
        if self.cnt[k] > 0 and self.seen[q].get(k, 0) < self.cnt[k]:
            self.streams[q].append(('wait', k, self.cnt[k]))
            self.seen[q][k] = self.cnt[k]
        self.cnt[k] += 16
        if fn is None:
            fn = (lambda e, out=out, in_=in_, kw=kw: e.dma_start(out=out, in_=in_, **kw))
        else:
            fn = fn(_PROXY)
        self.streams[q].append(('op', fn, k, 16))
        self._commit((k, self.cnt[k]), reads, writes)
        self.nops += 1

    def finish(self):
        st = self.streams['sp']
        for k, v in self.cnt.items():
            if v > 0 and k != 'sp' and self.seen['sp'].get(k, 0) < v:
                st.append(('wait', k, v))
                if k in self.marked:
                    self.marked[k].add(v)
        rank = {}
        for e in ENGS:
            rank[e] = {v: i + 1 for i, v in enumerate(sorted(self.marked[e]))}
        self.rank = rank
        nc = self.nc
        emap = {'pe': 'tensor', 'act': 'scalar', 'dve': 'vector', 'pool': 'gpsimd', 'sp': 'sync'}
        with nc.Block() as block:
            for e in ENGS:
                stream = self.streams[e]
                if not stream:
                    continue
                def body(engine, stream=stream):
                    for it in stream:
                        if it[0] == 'wait':
                            k, v = it[1], it[2]
                            if k in self.rank:
                                v = self.rank[k][v]
                            engine.wait_ge(self.sems[k], v)
                        else:
                            ins = it[1](engine)
                            k = it[2]
                            if k in self.rank:
                                if it[3] in self.rank[k]:
                                    ins.then_inc(self.sems[k], 1)
                            else:
                                ins.then_inc(self.sems[k], it[3])
                getattr(block, emap[e])(body)
        self.es.close()


def K(name, *idx):
    out = [(name,)]
    for ix in idx:
        if isinstance(ix, (list, tuple, range)):
            out = [o + (i,) for o in out for i in ix]
        else:
            out = [o + (ix,) for o in out]
    return out


def _barrier(self, skip_pool=False):
    for e in ENGS:
        if skip_pool and e == 'pool':
            continue
        for k, v in self.cnt.items():
            if skip_pool and (k == 'pool' or (isinstance(k, tuple) and k[1] == 'pool')):
                continue
            if v > 0 and k != e and self.seen[e].get(k, 0) < v:
                self.streams[e].append(('wait', k, v))
                self.seen[e][k] = v
                if k in self.marked:
                    self.marked[k].add(v)
Prog.barrier = _barrier


import numpy as np
import ml_dtypes

D = 1024; S = 2048; CTX = 256; DIN = 2816; NL = 2
EPS = 1e-6
BF = ml_dtypes.bfloat16
NARENA = 36896
NSLOT = 1152


class Unit:
    def __init__(self, kind, b, nb):
        self.kind = kind; self.b = b
        self.L = S if kind == 'lat' else CTX
        self.NT = min(512, self.L); self.nq = self.L // self.NT
        self.v = b if kind == 'lat' else nb
        self.cap = self.L // 8


class MK:
    def __init__(self, nb=4, nl=2):
        self.nb = nb; self.nl = nl
        self.nv = nb + 1
        self.nc = nc = bass.Bass("TRN2", target_bir_lowering=False)
        self.p = Prog(nc)
        self.dram = {}
        self.evi = 0; self.pbank = 0; self.wbi = 0; self.fsi = 0

    def din(self, name, shape, dtype=F32):
        self.dram[name] = self.nc.dram_tensor(name, list(shape), dtype, kind="ExternalInput").ap()
        return self.dram[name]
    def dout(self, name, shape, dtype=F32):
        self.dram[name] = self.nc.dram_tensor(name, list(shape), dtype, kind="ExternalOutput").ap()
        return self.dram[name]
    def dscr(self, name, shape, dtype=F32):
        self.dram[name] = self.nc.dram_tensor(name, list(shape), dtype).ap()
        return self.dram[name]

    def bv(self, off, n, dtype=BF16):
        if dtype == BF16:
            return self.big[:, off:off + n]
        assert off % 2 == 0
        return self.big[:, off:off + 2 * n].bitcast(dtype)

    def av(self, off, n, dtype=BF16):
        if dtype == BF16:
            return self.arena[:, off:off + n]
        assert off % 2 == 0
        return self.arena[:, off:off + 2 * n].bitcast(dtype)

    def declare(self):
        nb, nl, nv = self.nb, self.nl, self.nv
        p = self.p
        self.x = self.din("x", [nb, S, D])
        self.ctx = self.din("ctx", [nb, CTX, D])
        self.cT = self.din("cT", [128, 8, nv])
        self.w_mod = self.din("w_mod", [nl, D, 6 * D])
        self.b_modT = self.din("b_modT", [128, nl, 48])
        self.ngT = self.din("ngT", [128, 2 * nl + 1, 8])
        self.w_in = self.din("w_in", [nl, D, DIN])
        self.w_out = self.din("w_out", [nl, D, D])
        self.cst_f = self.din("cst_f", [128, 128 + 512 + 16 + 64 + 1])
        self.ident_b = self.din("ident_b", [128, 128], BF16)
        self.na_tab = self.din("na_tab", [nl, 128, 8, 9, 128])
        self.XS = self.dscr("XS", [nb, 4, 128, 8, 512])
        self.XCS = self.dscr("XCS", [nb, 1, 128, 8, 256])
        self.WB = self.dscr("WB", [nl, 8, 128, 8, 512], BF16)
        self.cst = p.sbuf("cst", [128, 128 + 512 + 16 + 64 + 1], F32)
        self.identf = self.cst[:, 0:128]
        self.iota512 = self.cst[:, 128:640]
        self.tokid = self.cst[:, 640:656]
        self.rowoff = self.cst[:, 656:720]
        self.poff = self.cst[:, 720:721]
        self.onesb = p.sbuf("onesb", [128, 128], BF16)
        self.identb = p.sbuf("identb", [128, 128], BF16)
        self.onesf = p.sbuf("onesf", [128, 128], F32)
        self.cTs = p.sbuf("cTs", [128, 8, nv], F32)
        self.mods = p.sbuf("mods", [128, 48, nv], F32)
        self.G = p.sbuf("G", [128, 2, 8, nv], F32)
        self.bmod = p.sbuf("bmod", [128, nl, 48], F32)
        self.ng = p.sbuf("ng", [128, 2 * nl + 1, 8], F32)
        self.epsc = p.sbuf("epsc", [128, 1], F32)
        self.g2p = p.sbuf("g2p", [128, 8, nv], F32)
        self.psall = p.psum("psall", [128, 7 * 512], F32)
        self.ps = [self.psall[:, i * 512:(i + 1) * 512] for i in range(7)]
        self.psb = p.psum("psb", [128, 1024], BF16)
        self.NW = 2
        self.wb = [p.sbuf("wb%d" % i, [128, 8, 512], BF16) for i in range(self.NW)]
        self.fs = [p.sbuf("fs%d" % i, [128, 8, 512], F32) for i in range(2)]
        self.sqb = [p.sbuf("sqb%d" % i, [128, 512], F32) for i in range(2)]
        self.ntb = [p.sbuf("ntb%d" % i, [128, 512], F32) for i in range(2)]
        self.tmpA = p.sbuf("tmpA", [128, 512], F32)
        self.tmpB = p.sbuf("tmpB", [128, 512], F32)
        self.kcT = p.sbuf("kcT", [128, 4, CTX], BF16)
        self.vc = p.sbuf("vc", [128, 2, 8, 65], BF16)
        self.big = p.sbuf("big", [128, 32768 + NARENA], BF16)
        self.hT = self.big[:, 0:16384].rearrange("p (c s) -> p c s", c=8)
        self.catT = self.big[:, 16384:32768].rearrange("p (c s) -> p c s", c=8)
        self.arena = self.big[:, 32768:32768 + NARENA]
        self.xtm = self.av(0, 2048, F32).rearrange("p (a b) -> p a b", a=2)
        a = 0
        self.qT = self.av(a, 8192).rearrange("p (c s) -> p c s", c=4); a += 8192
        self.kT = self.av(a, 8192).rearrange("p (c s) -> p c s", c=4); a += 8192
        self.v = self.av(a, 8320).rearrange("p (t h d) -> p t h d", t=16, h=8); a += 8320
        self.EB = self.av(a, 9216).rearrange("p (h t q) -> p h t q", h=8, t=9); a += 9216
        self.PT = [self.av(a + i * 896, 896) for i in range(2)]; a += 1792
        self.attn_tm = self.av(a, 512); a += 512
        self.rinv = self.av(a, 8, F32); a += 16
        assert a <= NARENA

    def init_consts(self):
        p = self.p
        p.dma('sp', self.cst[:], self.cst_f, writes=K('cst'))
        p.dma('sp', self.identb[:], self.ident_b, writes=K('identb'))
        p.dma('sp', self.cTs[:], self.cT, writes=K('cTs'))
        p.dma('sp', self.bmod[:], self.b_modT, writes=K('bmod'))
        p.dma('sp', self.ng[:], self.ngT, writes=K('ng'))
        p.op('dve', lambda e: e.memset(self.onesf[:], 1.0), writes=K('onesf'))
        p.op('dve', lambda e: e.memset(self.onesb[:], 1.0), writes=K('onesb'))
        p.op('dve', lambda e: e.memset(self.epsc[:], EPS), writes=K('epsc'))
        p.op('dve', lambda e: e.memset(self.vc[:], 1.0), writes=K('vc'))
        p.op('act', lambda e: e.activation(out=self.cTs[:], in_=self.cTs[:], func=AF.Silu),
             reads=K('cTs'), writes=K('cTs'))

    def evac(self, out, in_, reads, writes, eng=None):
        p = self.p
        if eng is None:
            eng = 'act' if (self.evi % 2) else 'dve'
            self.evi += 1
        if eng == 'act':
            p.op('act', lambda e: e.activation(out=out, in_=in_, func=AF.Copy), reads=reads, writes=writes)
        else:
            p.op('dve', lambda e: e.tensor_copy(out=out, in_=in_), reads=reads, writes=writes)

    def load_w(self, src):
        i = self.wbi; self.wbi = (self.wbi + 1) % self.NW
        a, b = src.shape[1], src.shape[2]
        dst = self.wb[i].rearrange("p a b -> p (a b)")[:, 0:a * b].rearrange("p (a b) -> p a b", a=a)
        self.p.dma('pool', dst, src, writes=K('wb', i))
        return dst, K('wb', i)

    WPIECE = {0: 0, 512: 1, 1024: 2, 1536: 3, 2048: 4, 2304: 5}

    def load_wb(self, l, idx, ncols=512):
        i = self.wbi; self.wbi = (self.wbi + 1) % self.NW
        dst = self.wb[i][:, :, 0:ncols]
        self.p.dma('pool', dst, self.WB[l, idx][:, :, 0:ncols], reads=K('WB', l, idx), writes=K('wb', i))
        return dst, K('wb', i)

    def convert_weights(self, l):
        p = self.p
        for c0, idx in self.WPIECE.items():
            nc_ = min(512, DIN - c0)
            wbuf, wk = self.load_w(self.w_in[l, :, c0:c0 + nc_].rearrange("(kc p) c -> p kc c", p=128))
            p.dma('sp', self.WB[l, idx][:, :, 0:nc_], wbuf, reads=wk, writes=K('WB', l, idx))
        for half in range(2):
            wbuf, wk = self.load_w(self.w_out[l, :, half * 512:(half + 1) * 512].rearrange("(kc p) c -> p kc c", p=128))
            p.dma('sp', self.WB[l, 6 + half], wbuf, reads=wk, writes=K('WB', l, 6 + half))

    def nbank(self):
        b = self.pbank; self.pbank = (self.pbank + 1) % 4
        return b

    def nfs(self):
        i = self.fsi; self.fsi ^= 1
        return self.fs[i], K('fs', i)

    def adaln(self, l):
        p = self.p; nv = self.nv
        for piece in range(12):
            st, sk = self.nfs()
            src = self.w_mod[l, :, piece * 512:(piece + 1) * 512].rearrange("(kc p) c -> p kc c", p=128)
            p.dma('sp', st[:], src, writes=sk)
            ps = self.ps[5]
            for j in range(4):
                for kc in range(8):
                    p.op('pe', lambda e, j=j, kc=kc, st=st: e.matmul(
                        ps[:, j * 8:j * 8 + nv], lhsT=st[:, kc, j * 128:(j + 1) * 128], rhs=self.cTs[:, kc, :],
                        start=(kc == 0), stop=(kc == 7)),
                        reads=sk + K('cTs'), writes=K('ps', 5))
            for j in range(4):
                oc = piece * 4 + j
                p.op('dve', lambda e, j=j, oc=oc: e.tensor_scalar(
                    out=self.mods[:, oc, :], in0=ps[:, j * 8:j * 8 + nv], scalar1=self.bmod[:, l, oc:oc + 1],
                    scalar2=None, op0=ALU.add), reads=K('ps', 5) + K('bmod'), writes=K('mods'))
        for which, m in ((0, 1), (1, 4)):
            gi = l if which == 0 else self.nl + l
            for kc in range(8):
                p.op('dve', lambda e, which=which, m=m, kc=kc, gi=gi: e.tensor_scalar(
                    out=self.G[:, which, kc, :], in0=self.mods[:, m * 8 + kc, :], scalar1=1.0,
                    scalar2=self.ng[:, gi, kc:kc + 1], op0=ALU.add, op1=ALU.mult),
                    reads=K('mods') + K('ng'), writes=K('G'))

    def norm_mod(self, xc, xkeys, n, which, v, outf, outkeys):
        p = self.p
        ps = self.ps[6]
        for kc in range(8):
            sq = self.sqb[kc % 2].bitcast(BF16); sqk = K('sqb', kc % 2)
            p.op('act', lambda e, kc=kc, sq=sq: e.activation(out=sq[:, 0:n], in_=xc[:, kc, :], func=AF.Square),
                 reads=xkeys, writes=sqk)
            p.op('pe', lambda e, kc=kc, sq=sq: e.matmul(ps[:, 0:n], lhsT=self.onesb[:], rhs=sq[:, 0:n],
                                                        start=(kc == 0), stop=(kc == 7)),
                 reads=sqk + K('onesb'), writes=K('ps', 6))
        p.op('act', lambda e: e.activation(out=self.tmpA[:, 0:n], in_=ps[:, 0:n], func=AF.Sqrt,
                                           bias=self.epsc[:, 0:1], scale=1.0 / D),
             reads=K('ps', 6) + K('epsc'), writes=K('tmpA'))
        p.op('dve', lambda e: e.reciprocal(out=self.tmpB[:, 0:n], in_=self.tmpA[:, 0:n]),
             reads=K('tmpA'), writes=K('tmpB'))
        for kc in range(8):
            nt = self.ntb[kc % 2]; ntk = K('ntb', kc % 2)
            p.op('dve', lambda e, kc=kc, nt=nt: e.tensor_tensor(out=nt[:, 0:n], in0=xc[:, kc, :], in1=self.tmpB[:, 0:n],
                                                               op=ALU.mult),
                 reads=xkeys + K('tmpB'), writes=ntk)
            if which == 2:
                sc = self.ng[:, 2 * self.nl, kc:kc + 1]; bi = 0.0
            else:
                sc = self.G[:, which, kc, v:v + 1]
                bi = self.mods[:, (0 if which == 0 else 3) * 8 + kc, v:v + 1]
            p.op('act', lambda e, kc=kc, nt=nt, sc=sc, bi=bi: e.activation(out=outf(kc), in_=nt[:, 0:n], func=AF.Identity,
                                                                           scale=sc, bias=bi),
                 reads=ntk + K('G') + K('mods') + K('ng'), writes=outkeys)

    def xs_ap(self, u, tq):
        return (self.XS if u.kind == 'lat' else self.XCS)[u.b, tq][:, :, 0:u.NT]
    def xs_key(self, u, tq):
        return K('XS' + u.kind, u.b, tq)

    def load_x_input(self, u, tq, xc, xk):
        p = self.p
        src = self.x if u.kind == 'lat' else self.ctx
        for tt in range(u.NT // 128):
            tok0 = tq * u.NT + tt * 128
            xt = self.xtm[:, tt % 2, :]; xtk = K('xtm', tt % 2)
            p.dma('sp', xt, src[u.b, tok0:tok0 + 128, :], writes=xtk)
            for half in range(2):
                bank = self.nbank()
                for j in range(4):
                    kc = half * 4 + j
                    p.op('pe', lambda e, kc=kc, j=j, bank=bank, xt=xt: e.transpose(
                        out=self.ps[bank][:, j * 128:(j + 1) * 128], in_=xt[:, kc * 128:(kc + 1) * 128],
                        identity=self.identf), reads=xtk + K('cst'), writes=K('ps', bank))
                self.evac(xc[:, half * 4:half * 4 + 4, tt * 128:(tt + 1) * 128],
                          self.ps[bank].rearrange("p (j t) -> p j t", j=4), K('ps', bank), xk)

    def stage1(self, l, u, pre=None):
        p = self.p
        def fetch(tq):
            fsb, xk = self.nfs()
            xc = fsb[:, :, 0:u.NT]
            if l == 0:
                self.load_x_input(u, tq, xc, xk)
                p.dma('sp', self.xs_ap(u, tq), xc, reads=xk, writes=self.xs_key(u, tq))
            else:
                p.dma('sp', xc, self.xs_ap(u, tq), reads=self.xs_key(u, tq), writes=xk)
                if pre is not None:
                    pre(tq, xc, xk)
                    p.dma('sp', self.xs_ap(u, tq), xc, reads=xk, writes=self.xs_key(u, tq))
            return xc, xk
        cur = fetch(0)
        for tq in range(u.nq):
            nxt = fetch(tq + 1) if tq + 1 < u.nq else None
            xc, xk = cur
            self.norm_mod(xc, xk, u.NT, 0, u.v,
                          lambda kc, tq=tq: self.hT[:, kc, tq * u.NT:(tq + 1) * u.NT], K('hT', tq))
            cur = nxt

    def proj_fm(self, l, col0, nblk, u, evac):
        p = self.p
        NT = u.NT
        for pc in range(0, nblk, 4):
            nb_ = min(4, nblk - pc)
            c0 = col0 + pc * 128
            wb, wk = self.load_wb(l, self.WPIECE[c0], nb_ * 128)
            for ml in range(nb_):
                for tq in range(u.nq):
                    bank = self.nbank()
                    for kc in range(8):
                        p.op('pe', lambda e, kc=kc, ml=ml, tq=tq, bank=bank, wb=wb: e.matmul(
                            self.ps[bank][:, 0:NT], lhsT=wb[:, kc, ml * 128:(ml + 1) * 128],
                            rhs=self.hT[:, kc, tq * NT:(tq + 1) * NT], start=(kc == 0), stop=(kc == 7)),
                            reads=wk + K('hT', tq), writes=K('ps', bank))
                    evac(pc + ml, tq, self.ps[bank][:, 0:NT], K('ps', bank))

    def proj_v(self, l, u, vdst, vkeys):
        p = self.p
        wb, wk = self.load_wb(l, 2)
        for tt in range(u.L // 128):
            bank = self.nbank()
            for kc in range(8):
                p.op('pe', lambda e, kc=kc, tt=tt, bank=bank: e.matmul(
                    self.ps[bank], lhsT=self.hT[:, kc, tt * 128:(tt + 1) * 128], rhs=wb[:, kc, :],
                    start=(kc == 0), stop=(kc == 7)),
                    reads=wk + K('hT', (tt * 128) // u.NT), writes=K('ps', bank))
            self.evac(vdst[:, tt, :, 0:64], self.ps[bank].rearrange("p (h d) -> p h d", d=64),
                      K('ps', bank), vkeys(tt))

    def load_na_tables(self, l):
        p = self.p
        for h in range(8):
            st, sk = self.nfs()
            stv = st.rearrange("p a b -> p (a b)")[:, 0:9 * 128]
            p.dma('sp', stv, self.na_tab[l, :, h].rearrange("p t q -> p (t q)"), writes=sk)
            p.op('act', lambda e, h=h, stv=stv: e.activation(
                out=self.EB[:, h].rearrange("p t q -> p (t q)"), in_=stv, func=AF.Copy, scale=8.0),
                reads=sk, writes=K('EB'))

    def attention(self, qT, qkeys, blocks, kT, v, out_catT):
        p = self.p
        items = [(bi, h) for bi in range(len(blocks)) for h in range(8)]
        def qk(k):
            bi, h = items[k]
            q0, local = blocks[bi]
            nloc = len(local); ntot = nloc + 2
            hp, r0 = h // 2, (h % 2) * 64
            sb = 2 + 2 * (k % 2)
            ST = self.psall[:, sb * 512: sb * 512 + 1024]
            stk = K('ps', sb) + K('ps', sb + 1)
            for n in range(ntot):
                if n < nloc:
                    j = local[n][0]
                    lhsT = kT[r0:r0 + 64, hp, j * 128:(j + 1) * 128]; lk = K('kT', j // 4)
                else:
                    jc = n - nloc
                    lhsT = self.kcT[r0:r0 + 64, hp, jc * 128:(jc + 1) * 128]; lk = K('kcT')
                p.op('pe', lambda e, n=n, lhsT=lhsT: e.matmul(
                    ST[:, n * 128:(n + 1) * 128], lhsT=lhsT, rhs=qT[r0:r0 + 64, hp, q0:q0 + 128],
                    start=(n % 4 == 0), stop=False), reads=lk + qkeys(q0), writes=[stk[n // 4]])
            tids = [t for (_, t) in local]
            runs = []
            for n, t in enumerate(tids):
                if runs and runs[-1][1] + runs[-1][2] == t and (n // 4) == (runs[-1][0] // 4):
                    runs[-1][2] += 1
                else:
                    runs.append([n, t, 1])
            for n0_, t0_, ln in runs:
                p.op('pe', lambda e, n0_=n0_, t0_=t0_, ln=ln: e.matmul(
                    ST[:, n0_ * 128:(n0_ + ln) * 128], lhsT=self.identb[:],
                    rhs=self.EB[:, h, t0_:t0_ + ln, :].rearrange("p t q -> p (t q)"), start=False, stop=True),
                    reads=K('EB') + K('identb'), writes=[stk[n0_ // 4]])
        qk(0)
        pending = None
        for k, (bi, h) in enumerate(items):
            if k + 1 < len(items):
                qk(k + 1)
            q0, local = blocks[bi]
            nloc = len(local); ntot = nloc + 2
            sb = 2 + 2 * (k % 2); pb = k % 2
            ST = self.psall[:, sb * 512: sb * 512 + 1024]
            stk = K('ps', sb) + K('ps', sb + 1)
            pt = self.PT[pb]
            p.op('act', lambda e, ST=ST, pt=pt, ntot=ntot: e.activation(
                out=pt[:, 0:ntot * 128], in_=ST[:, 0:ntot * 128], func=AF.Exp, scale=0.125),
                reads=stk, writes=K('PT', pb, range(ntot)))
            ob = h // 4; oc0 = (h % 4) * 65
            for n in range(ntot):
                if n < nloc:
                    j = local[n][0]; rhs = v[:, j, h, :]; rk = K('v', j)
                else:
                    jc = n - nloc; rhs = self.vc[:, jc, h, :]; rk = K('vc')
                p.op('pe', lambda e, n=n, rhs=rhs, pt=pt, ob=ob, oc0=oc0, ntot=ntot: e.matmul(
                    self.ps[ob][:, oc0:oc0 + 65], lhsT=pt[:, n * 128:(n + 1) * 128], rhs=rhs,
                    start=(n == 0), stop=(n == ntot - 1)), reads=K('PT', pb, n) + rk, writes=K('ps', ob))
            if pending is not None and h == 0:
                pending(); pending = None
            if h == 7:
                for ob in range(2):
                    p.op('dve', lambda e, ob=ob: e.reciprocal(
                        out=self.rinv[:, ob * 4:(ob + 1) * 4],
                        in_=self.ps[ob][:, 0:260].rearrange("p (h d) -> p h d", d=65)[:, :, 64]),
                        reads=K('ps', ob), writes=K('rinv', ob))
                for hh in range(8):
                    ob = hh // 4; oc0 = (hh % 4) * 65
                    p.op('dve', lambda e, hh=hh, ob=ob, oc0=oc0: e.tensor_scalar(
                        out=self.attn_tm[:, hh * 64:(hh + 1) * 64], in0=self.ps[ob][:, oc0:oc0 + 64],
                        scalar1=self.rinv[:, hh:hh + 1], scalar2=None, op0=ALU.mult),
                        reads=K('ps', ob) + K('rinv', ob), writes=K('attn_tm', hh // 2))
                def fin(bi=bi):
                    for c4 in range(4):
                        p.op('pe', lambda e, c4=c4: e.transpose(out=self.psb[:, c4 * 128:(c4 + 1) * 128],
                                                               in_=self.attn_tm[:, c4 * 128:(c4 + 1) * 128], identity=self.identb[:]),
                             reads=K('attn_tm', c4) + K('identb'), writes=K('psb'))
                    out_ap, out_keys = out_catT(bi)
                    self.evac(out_ap, self.psb[:, 0:512].rearrange("p (c q) -> p c q", c=4), K('psb'), out_keys, eng='dve')
                pending = fin
        if pending is not None:
            pending()

    def attn_stage(self, l, u, sched, last):
        p = self.p
        NT = u.NT
        if u.kind == 'ctx':
            self.proj_fm(l, 512, 4, u, lambda m, tq, ps, pk: self.evac(self.kcT[:, m, :], ps, pk, K('kcT')))
            self.proj_v(l, u, self.vc, lambda tt: K('vc'))
            if last:
                return
            self.proj_fm(l, 0, 4, u, lambda m, tq, ps, pk: self.evac(self.qT[:, m, 0:CTX], ps, pk, K('qT', 0)))
            blocks = [(i * 128, []) for i in range(2)]
            self.attention(self.qT, lambda q0: K('qT', 0), blocks, None, None,
                           lambda bi: (self.catT[:, 0:4, bi * 128:(bi + 1) * 128], K('catT', 0)))
        else:
            p.op('dve', lambda e: e.memset(self.v[:, :, :, 64:65], 1.0), writes=K('v', range(16)))
            self.load_na_tables(l)
            self.proj_fm(l, 0, 4, u, lambda m, tq, ps, pk: self.evac(self.qT[:, m, tq * NT:(tq + 1) * NT], ps, pk, K('qT', tq)))
            self.proj_fm(l, 512, 4, u, lambda m, tq, ps, pk: self.evac(self.kT[:, m, tq * NT:(tq + 1) * NT], ps, pk, K('kT', tq)))
            self.proj_v(l, u, self.v, lambda tt: K('v', tt))
            blocks = [(i * 128, sched[i]) for i in range(16)]
            self.attention(self.qT, lambda q0: K('qT', q0 // 512), blocks, self.kT, self.v,
                           lambda bi: (self.catT[:, 0:4, bi * 128:(bi + 1) * 128], K('catT', bi // 4)))


TWO_PI = 2.0 * np.pi

def _declare_hyena(self):
    nl = self.nl
    self.dftA = {}; self.dftB = {}; self.zpos = {}; self.decay = {}; self.KS = {}
    for L in (2048, 256):
        nC = L // 128; NT = min(512, L); nq = L // NT; FG = min(4, nC); nfg = nC // FG
        self.dftA[L] = self.din("dftA%d" % L, [nC, 128, 2, nC, 128], BF16)
        self.dftB[L] = self.din("dftB%d" % L, [nq, nfg, 128, 2, FG, NT], BF16)
        self.zpos[L] = self.din("zpos%d" % L, [64, L])
        self.decay[L] = self.din("decay%d" % L, [128, nC, 2, 256])
        self.KS[L] = self.dscr("KS%d" % L, [nC, 128, 2, 512])
    self.hyw_d = self.din("hyw", [nl, 64, 1156])
    self.hysc_d = self.din("hysc", [128, nl, 28])

    self.hysc = self.p.sbuf("hysc_s", [128, nl, 28], F32)
    self.p.dma('sp', self.hysc[:], self.hysc_d, writes=K('hysc'))
MK.declare_hyena = _declare_hyena

def _sin_layer(self, src_ps, n, bcol, fcol, dst):
    p = self.p
    A = self.tmpA[0:64, 0:n]; Bt = self.tmpB[0:64, 0:n]
    p.op('dve', lambda e: e.tensor_scalar(out=A, in0=src_ps, scalar1=bcol, scalar2=fcol, op0=ALU.add, op1=ALU.mult),
         reads=K('ps', 4) + K('hyw'), writes=K('tmpA'))
    p.op('dve', lambda e: e.tensor_scalar(out=Bt, in0=A, scalar1=float(np.pi), scalar2=-TWO_PI, op0=ALU.is_gt, op1=ALU.mult),
         reads=K('tmpA'), writes=K('tmpB'))
    p.op('dve', lambda e: e.tensor_tensor(out=A, in0=A, in1=Bt, op=ALU.add), reads=K('tmpA') + K('tmpB'), writes=K('tmpA'))
    p.op('dve', lambda e: e.tensor_scalar(out=Bt, in0=A, scalar1=-float(np.pi), scalar2=TWO_PI, op0=ALU.is_lt, op1=ALU.mult),
         reads=K('tmpA'), writes=K('tmpB'))
    p.op('dve', lambda e: e.tensor_tensor(out=A, in0=A, in1=Bt, op=ALU.add), reads=K('tmpA') + K('tmpB'), writes=K('tmpA'))
    p.op('act', lambda e: e.activation(out=dst, in_=A, func=AF.Sin), reads=K('tmpA'), writes=K('fs', 0) + K('fs', 1))
MK.sin_layer = _sin_layer

def _hyena_filter(self, l, L):
    p = self.p
    nC = L // 128; NT = min(512, L); nq = L // NT
    fs0 = self.fs[0].rearrange("p a b -> p (a b)"); fs1 = self.fs[1].rearrange("p a b -> p (a b)")
    zp = fs0[0:64, 0:L]; h1 = fs0[0:64, L:2 * L]; h2 = fs1[0:64, 0:L]
    kp = self.av(0, 8192).rearrange("p (t n) -> p t n", t=16)
    km = self.av(8192, 8192).rearrange("p (t n) -> p t n", t=16)
    hyw = self.av(16384, 1156, F32)[0:64, :]
    dcb = [self.av(16384 + 2320 + i * 1024, 512, F32).rearrange("p (a b) -> p a b", a=2) for i in range(2)]
    abuf = [self.av(16384 + 2320 + 2048 + i * 4096, 4096).rearrange("p (c t f) -> p c t f", c=2, t=16) for i in range(2)]
    p.dma('sp', zp, self.zpos[L], writes=K('fs', 0))
    p.dma('sp', hyw, self.hyw_d[l], writes=K('hyw'))
    w1 = hyw[:, 0:64]; w2 = hyw[:, 64:128]; w3 = hyw[:, 128:1152]
    for tq in range(nq):
        ps = self.ps[4][0:64, 0:NT]
        p.op('pe', lambda e, tq=tq: e.matmul(ps, lhsT=w1, rhs=zp[:, tq * NT:(tq + 1) * NT], start=True, stop=True),
             reads=K('hyw') + K('fs', 0), writes=K('ps', 4))
        self.sin_layer(ps, NT, hyw[:, 1152:1153], hyw[:, 1153:1154], h1[:, tq * NT:(tq + 1) * NT])
    for tq in range(nq):
        ps = self.ps[4][0:64, 0:NT]
        p.op('pe', lambda e, tq=tq: e.matmul(ps, lhsT=w2, rhs=h1[:, tq * NT:(tq + 1) * NT], start=True, stop=True),
             reads=K('hyw') + K('fs', 0), writes=K('ps', 4))
        self.sin_layer(ps, NT, hyw[:, 1154:1155], hyw[:, 1153:1154], h2[:, tq * NT:(tq + 1) * NT])
    for tc in range(nC):
        dc = dcb[tc % 2]; dk = K('dcb', tc % 2)
        p.dma('sp', dc, self.decay[L][:, tc], writes=dk)
        for half in range(2):
            p.op('pe', lambda e, tc=tc, half=half: e.matmul(
                self.ps[half], lhsT=h2[:, tc * 128:(tc + 1) * 128], rhs=w3[:, half * 512:(half + 1) * 512],
                start=True, stop=True), reads=K('hyw') + K('fs', 1), writes=K('ps', half))
        fw = self.ntb[0]; bw = self.ntb[1]
        for o in range(2):
            p.op('dve', lambda e, o=o, dc=dc: e.tensor_tensor(out=fw[:, o * 256:(o + 1) * 256], in0=self.ps[0][:, o * 256:(o + 1) * 256],
                                                            in1=dc[:, 0, :], op=ALU.mult), reads=K('ps', 0) + dk, writes=K('ntb', 0))
            p.op('dve', lambda e, o=o, dc=dc: e.tensor_tensor(out=bw[:, o * 256:(o + 1) * 256], in0=self.ps[1][:, o * 256:(o + 1) * 256],
                                                            in1=dc[:, 1, :], op=ALU.mult), reads=K('ps', 1) + dk, writes=K('ntb', 1))
        p.op('dve', lambda e, tc=tc: e.tensor_tensor(out=kp[:, tc, :], in0=fw[:], in1=bw[:], op=ALU.add),
             reads=K('ntb', 0) + K('ntb', 1), writes=K('kp'))
        p.op('dve', lambda e, tc=tc: e.tensor_tensor(out=km[:, tc, :], in0=fw[:], in1=bw[:], op=ALU.subtract),
             reads=K('ntb', 0) + K('ntb', 1), writes=K('km'))
    for fc in range(nC):
        ab = abuf[fc % 2][:, :, 0:nC, :]; ak = K('abuf', fc % 2)
        p.dma('sp', ab, self.dftA[L][fc], writes=ak)
        b0 = 2 * (fc % 2)
        for tc in range(nC):
            p.op('pe', lambda e, tc=tc, ab=ab, b0=b0: e.matmul(self.ps[b0], lhsT=ab[:, 0, tc, :], rhs=kp[:, tc, :],
                                                            start=(tc == 0), stop=(tc == nC - 1)),
                 reads=ak + K('kp'), writes=K('ps', b0))
            p.op('pe', lambda e, tc=tc, ab=ab, b0=b0: e.matmul(self.ps[b0 + 1], lhsT=ab[:, 1, tc, :], rhs=km[:, tc, :],
                                                            start=(tc == 0), stop=(tc == nC - 1)),
                 reads=ak + K('km'), writes=K('ps', b0 + 1))
        for cs in range(2):
            self.evac(self.sqb[cs][:], self.ps[b0 + cs], K('ps', b0 + cs), K('sqb', cs))
            p.dma('sp', self.KS[L][fc, :, cs, :], self.sqb[cs][:], reads=K('sqb', cs), writes=K('KS', L, fc))
MK.hyena_filter = _hyena_filter

def _hyena_unit(self, l, u):
    p = self.p
    L = u.L; NT = u.NT; nq = u.nq; nC = L // 128; FG = min(4, nC); nfg = nC // FG
    a = 0
    xg = [self.av(a + i * 4096, 4096).rearrange("p (c s) -> p c s", c=2) for i in range(3)]; a += 12288
    ub = [self.av(a, 2064) for i in range(2)]; a += 2064
    ztok = self.av(a, 4096).rearrange("p (t n) -> p t n", t=16); a += 4096
    Yre = self.av(a, 4096).rearrange("p (f n) -> p f n", f=16); a += 4096
    Ys = self.av(a, 4096).rearrange("p (f n) -> p f n", f=16); a += 4096
    fsb_ = [self.fs[i].rearrange("p a b -> p (a b)").bitcast(BF16) for i in range(2)]
    slot = [fsb_[s // 2][:, (s % 2) * 4096:(s % 2 + 1) * 4096] for s in range(4)]
    ksb = [self.av(a + i * 2048, 1024, F32).rearrange("p (c n) -> p c n", c=2) for i in range(2)]; a += 4096
    assert a <= NARENA
    fs0 = self.fs[0].rearrange("p a b -> p (a b)"); fs1 = self.fs[1].rearrange("p a b -> p (a b)")
    sc = self.hysc
    for i in range(2):
        p.op('dve', lambda e, i=i: e.memset(ub[i][:, 0:1], 0.0), writes=K('ub', i))
        p.op('dve', lambda e, i=i: e.memset(ub[i][:, L + 1:L + 2], 0.0), writes=K('ub', i))
    def ev(m, tq, ps, pk):
        ubm = ub[0]; uk = K('ub', 0)
        self.evac(ubm[:, 1 + tq * NT:1 + (tq + 1) * NT], ps, pk, uk)
        if tq == nq - 1:
            dest = xg[m // 2][:, m % 2, 0:L]; dk = K('xg', m // 2)
            T1 = fs0[:, 0:L]; T2 = fs1[:, 0:L]
            p.op('dve', lambda e: e.tensor_scalar(out=T1, in0=ubm[:, 1:L + 1], scalar1=sc[:, l, m * 3 + 1:m * 3 + 2],
                                                  scalar2=sc[:, l, 18 + m:19 + m], op0=ALU.mult, op1=ALU.add),
                 reads=uk + K('hysc'), writes=K('fs', 0))
            p.op('dve', lambda e: e.scalar_tensor_tensor(out=T2, in0=ubm[:, 0:L], scalar=sc[:, l, m * 3:m * 3 + 1], in1=T1,
                                                         op0=ALU.mult, op1=ALU.add),
                 reads=uk + K('hysc') + K('fs', 0), writes=K('fs', 1))
            p.op('dve', lambda e: e.scalar_tensor_tensor(out=dest, in0=ubm[:, 2:L + 2], scalar=sc[:, l, m * 3 + 2:m * 3 + 3], in1=T2,
                                                         op0=ALU.mult, op1=ALU.add),
                 reads=uk + K('hysc') + K('fs', 1), writes=dk)
    self.proj_fm(l, 1536, 6, u, ev)
    p.barrier(True)
    z = xg[2]
    for o in range(2):
        gate = xg[o]
        for t4 in range(0, nC, 4):
            nt4 = min(4, nC - t4)
            for tl in range(nt4):
                for cch in range(2):
                    p.op('pe', lambda e, tl=tl, cch=cch, t4=t4: e.transpose(
                        out=self.psb[:, tl * 256 + cch * 128: tl * 256 + (cch + 1) * 128],
                        in_=z[:, cch, (t4 + tl) * 128:(t4 + tl + 1) * 128], identity=self.identb[:]),
                        reads=K('xg', 2) + K('identb'), writes=K('psb'))
            self.evac(ztok[:, t4:t4 + nt4, :], self.psb[:, 0:nt4 * 256].rearrange("p (t n) -> p t n", t=nt4),
                      K('psb'), K('ztok'))
        for fp in range(nC // 2):
            ak = K('fsh', 2 * (fp % 2)) + K('fsh', 2 * (fp % 2) + 1)
            abq = [slot[2 * (fp % 2) + q][:, 0:2 * nC * 128].rearrange("p (c t f) -> p c t f", c=2, t=nC) for q in range(2)]
            for q in range(2):
                p.dma('sp', abq[q], self.dftA[L][2 * fp + q], writes=K('fsh', 2 * (fp % 2) + q))
            ks = ksb[fp % 2]; kk = K('ksb', fp % 2)
            for q in range(2):
                p.dma('sp', ks[:, :, q * 256:(q + 1) * 256], self.KS[L][2 * fp + q, :, :, o * 256:(o + 1) * 256],
                      reads=K('KS', L, 2 * fp + q), writes=kk)
            b0 = 2 * (fp % 2)
            for q in range(2):
                for tc in range(nC):
                    p.op('pe', lambda e, tc=tc, q=q: e.matmul(self.ps[b0][:, q * 256:(q + 1) * 256], lhsT=abq[q][:, 0, tc, :], rhs=ztok[:, tc, :],
                                                             start=(tc == 0), stop=(tc == nC - 1)),
                         reads=ak + K('ztok'), writes=K('ps', b0))
                for tc in range(nC):
                    p.op('pe', lambda e, tc=tc, q=q: e.matmul(self.ps[b0 + 1][:, q * 256:(q + 1) * 256], lhsT=abq[q][:, 1, tc, :], rhs=ztok[:, tc, :],
                                                             start=(tc == 0), stop=(tc == nC - 1)),
                         reads=ak + K('ztok'), writes=K('ps', b0 + 1))
            Zr = self.ps[b0]; Zs = self.ps[b0 + 1]
            Kr = ks[:, 0, :]; Ksn = ks[:, 1, :]
            n0 = self.ntb[0]; n1 = self.ntb[1]; n2 = self.sqb[0]; n3 = self.sqb[1]
            yre = Yre[:, 2 * fp:2 * fp + 2, :].rearrange("p q n -> p (q n)")
            ysv = Ys[:, 2 * fp:2 * fp + 2, :].rearrange("p q n -> p (q n)")
            p.op('dve', lambda e: e.tensor_tensor(out=n0[:], in0=Zr, in1=Kr, op=ALU.mult), reads=K('ps', b0) + kk, writes=K('ntb', 0))
            p.op('dve', lambda e: e.tensor_tensor(out=n1[:], in0=Zs, in1=Ksn, op=ALU.mult), reads=K('ps', b0 + 1) + kk, writes=K('ntb', 1))
            p.op('dve', lambda e: e.tensor_tensor(out=yre, in0=n0[:], in1=n1[:], op=ALU.subtract),
                 reads=K('ntb', 0) + K('ntb', 1), writes=K('Yre'))
            p.op('dve', lambda e: e.tensor_tensor(out=n2[:], in0=Zr, in1=Ksn, op=ALU.mult), reads=K('ps', b0) + kk, writes=K('sqb', 0))
            p.op('dve', lambda e: e.tensor_tensor(out=n3[:], in0=Zs, in1=Kr, op=ALU.mult), reads=K('ps', b0 + 1) + kk, writes=K('sqb', 1))
            p.op('dve', lambda e: e.tensor_tensor(out=ysv, in0=n2[:], in1=n3[:], op=ALU.add),
                 reads=K('sqb', 0) + K('sqb', 1), writes=K('Ys'))
        bi = 0
        for tq in range(nq):
            b0 = 2 * (tq % 2)
            for fg in range(nfg):
                bb = slot[bi % 4][:, 0:2 * FG * NT].rearrange("p (c f t) -> p c f t", c=2, f=FG); bk = K('fsh', bi % 4); bi += 1
                p.dma('sp', bb, self.dftB[L][tq, fg], writes=bk)
                for fl in range(FG):
                    fc = fg * FG + fl
                    for cch in range(2):
                        p.op('pe', lambda e, fc=fc, fl=fl, cch=cch, bb=bb, b0=b0: e.matmul(
                            self.ps[b0 + cch][:, 0:NT], lhsT=Yre[:, fc, cch * 128:(cch + 1) * 128], rhs=bb[:, 0, fl, :],
                            start=(fc == 0), stop=False), reads=bk + K('Yre'), writes=K('ps', b0 + cch))
                        p.op('pe', lambda e, fc=fc, fl=fl, cch=cch, bb=bb, b0=b0: e.matmul(
                            self.ps[b0 + cch][:, 0:NT], lhsT=Ys[:, fc, cch * 128:(cch + 1) * 128], rhs=bb[:, 1, fl, :],
                            start=False, stop=(fc == nC - 1)), reads=bk + K('Ys'), writes=K('ps', b0 + cch))
            for cch in range(2):
                zc = z[:, cch, tq * NT:(tq + 1) * NT]
                n0 = self.ntb[cch][:, 0:NT]; nk = K('ntb', cch)
                n1 = self.sqb[cch][:, 0:NT]; sk = K('sqb', cch)
                dcol = sc[:, l, 24 + o * 2 + cch:25 + o * 2 + cch]
                p.op('dve', lambda e, zc=zc, n0=n0, dcol=dcol: e.tensor_scalar(out=n0, in0=zc, scalar1=dcol, scalar2=None, op0=ALU.mult),
                     reads=K('xg', 2) + K('hysc'), writes=nk)
                p.op('dve', lambda e, n0=n0, n1=n1, b0=b0, cch=cch: e.scalar_tensor_tensor(
                    out=n1, in0=self.ps[b0 + cch][:, 0:NT], scalar=1.0 / L, in1=n0, op0=ALU.mult, op1=ALU.add),
                    reads=K('ps', b0 + cch) + nk, writes=sk)
                if o == 0:
                    dst = zc; dk = K('xg', 2)
                else:
                    dst = self.catT[:, 4 + cch, tq * NT:(tq + 1) * NT]; dk = K('catT', tq)
                p.op('dve', lambda e, n1=n1, dst=dst, cch=cch, tq=tq, gate=gate: e.tensor_tensor(
                    out=dst, in0=n1, in1=gate[:, cch, tq * NT:(tq + 1) * NT], op=ALU.mult),
                    reads=sk + K('xg', o), writes=dk)
MK.hyena_unit = _hyena_unit


def _declare_cf(self):
    nl = self.nl; p = self.p; nb = self.nb
    self.cfsc_d = self.din("cfsc", [128, nl, 2, 34])
    self.cfsc = p.sbuf("cfsc_s", [128, nl, 2, 34], F32)
    p.dma('sp', self.cfsc[:], self.cfsc_d, writes=K('cfsc'))
    self.rw_d = self.din("router_wT", [128, nl, 8, 16])
    self.rw = p.sbuf("rw_s", [128, nl, 8, 16], F32)
    p.dma('sp', self.rw[:], self.rw_d, writes=K('rw'))
    self.H2S = self.dscr("H2S", [nb, 16, 128, 1024], BF16)
    self.H2CS = self.dscr("H2CS", [nb, 2, 128, 1024], BF16)
    self.AFFS = self.dscr("AFFS", [16 * nb, S])
    self.AFFCS = self.dscr("AFFCS", [16 * nb, CTX])
MK.declare_cf = _declare_cf

def _conformer_unit(self, l, u):
    p = self.p
    L = u.L; NT = u.NT; nq = u.nq
    LP = L + 30
    a = 0
    glu = self.av(a, 2 * LP).rearrange("p (c s) -> p c s", c=2); a += 2 * LP + (2 * LP) % 2
    diag = self.av(a, 7936).rearrange("p (c j f) -> p c j f", c=2, j=31); a += 7936
    atmp = self.av(a, 2 * L).rearrange("p (c s) -> p c s", c=2); a += 2 * L
    assert a <= NARENA
    cs = self.cfsc
    p.op('dve', lambda e: e.memset(glu[:, :, 0:15], 0.0), writes=K('glu'))
    p.op('dve', lambda e: e.memset(glu[:, :, L + 15:L + 30], 0.0), writes=K('glu'))
    for cch in range(2):
        for j in range(31):
            p.op('dve', lambda e, cch=cch, j=j: e.tensor_scalar(out=diag[:, cch, j, :], in0=self.identf,
                                                              scalar1=cs[:, l, cch, j:j + 1], scalar2=None, op0=ALU.mult),
                 reads=K('cst') + K('cfsc'), writes=K('diag'))
    def ev(m, tq, ps, pk):
        if m < 2:
            self.evac(atmp[:, m, tq * NT:(tq + 1) * NT], ps, pk, K('atmp'))
        else:
            cch = m - 2
            sg = self.ntb[cch][:, 0:NT]
            p.op('act', lambda e: e.activation(out=sg, in_=ps, func=AF.Sigmoid), reads=pk, writes=K('ntb', cch))
            p.op('dve', lambda e: e.tensor_tensor(out=glu[:, cch, 15 + tq * NT:15 + (tq + 1) * NT], in0=sg,
                                                  in1=atmp[:, cch, tq * NT:(tq + 1) * NT], op=ALU.mult),
                 reads=K('ntb', cch) + K('atmp'), writes=K('glu'))
    self.proj_fm(l, 2304, 4, u, ev)
    for tq in range(nq):
        yb = self.fs[0]; ysq = self.fs[1]
        for cch in range(2):
            bank = self.nbank()
            for j in range(31):
                p.op('pe', lambda e, cch=cch, j=j, bank=bank, tq=tq: e.matmul(
                    self.ps[bank][:, 0:NT], lhsT=diag[:, cch, j, :], rhs=glu[:, cch, tq * NT + j:tq * NT + j + NT],
                    start=(j == 0), stop=(j == 30)), reads=K('diag') + K('glu'), writes=K('ps', bank))
            p.op('dve', lambda e, cch=cch, bank=bank: e.tensor_scalar(out=yb[:, cch, 0:NT], in0=self.ps[bank][:, 0:NT],
                                                                    scalar1=cs[:, l, cch, 31:32], scalar2=None, op0=ALU.add),
                 reads=K('ps', bank) + K('cfsc'), writes=K('fs', 0))
            p.op('act', lambda e, cch=cch: e.activation(out=ysq[:, cch, 0:NT], in_=yb[:, cch, 0:NT], func=AF.Square),
                 reads=K('fs', 0), writes=K('fs', 1))
        for cch in range(2):
            p.op('pe', lambda e, cch=cch: e.matmul(self.ps[4][:, 0:NT], lhsT=self.onesf[:], rhs=yb[:, cch, 0:NT],
                                                  start=(cch == 0), stop=(cch == 1)), reads=K('fs', 0) + K('onesf'), writes=K('ps', 4))
        for cch in range(2):
            p.op('pe', lambda e, cch=cch: e.matmul(self.ps[5][:, 0:NT], lhsT=self.onesf[:], rhs=ysq[:, cch, 0:NT],
                                                  start=(cch == 0), stop=(cch == 1)), reads=K('fs', 1) + K('onesf'), writes=K('ps', 5))
        mean = self.tmpA[:, 0:NT]; tb = self.tmpB[:, 0:NT]
        p.op('dve', lambda e: e.tensor_scalar(out=mean, in0=self.ps[4][:, 0:NT], scalar1=1.0 / 256, scalar2=None, op0=ALU.mult),
             reads=K('ps', 4), writes=K('tmpA'))
        p.op('dve', lambda e: e.tensor_tensor(out=tb, in0=mean, in1=mean, op=ALU.mult), reads=K('tmpA'), writes=K('tmpB'))
        p.op('dve', lambda e: e.scalar_tensor_tensor(out=tb, in0=self.ps[5][:, 0:NT], scalar=1.0 / 256, in1=tb,
                                                     op0=ALU.mult, op1=ALU.subtract), reads=K('ps', 5) + K('tmpB'), writes=K('tmpB'))
        p.op('act', lambda e: e.activation(out=tb, in_=tb, func=AF.Sqrt, bias=self.epsc[:, 0:1], scale=1.0),
             reads=K('tmpB') + K('epsc'), writes=K('tmpB'))
        p.op('dve', lambda e: e.reciprocal(out=tb, in_=tb), reads=K('tmpB'), writes=K('tmpB'))
        for cch in range(2):
            t = self.ntb[cch][:, 0:NT]
            p.op('dve', lambda e, cch=cch, t=t: e.tensor_tensor(out=t, in0=yb[:, cch, 0:NT], in1=mean, op=ALU.subtract),
                 reads=K('fs', 0) + K('tmpA'), writes=K('ntb', cch))
            p.op('dve', lambda e, cch=cch, t=t: e.tensor_tensor(out=t, in0=t, in1=tb, op=ALU.mult),
                 reads=K('ntb', cch) + K('tmpB'), writes=K('ntb', cch))
            p.op('act', lambda e, cch=cch, t=t, tq=tq: e.activation(
                out=self.catT[:, 6 + cch, tq * NT:(tq + 1) * NT], in_=t, func=AF.Silu,
                scale=cs[:, l, cch, 32:33], bias=cs[:, l, cch, 33:34]),
                reads=K('ntb', cch) + K('cfsc'), writes=K('catT', tq))
MK.conformer_unit = _conformer_unit

def _stage5(self, l, u):
    p = self.p
    L = u.L; NT = u.NT; nq = u.nq; b = u.b
    h2b = self.av(0, 8 * NT).rearrange("p (c s) -> p c s", c=8)
    h2tm = [self.av(4096 + i * 1024, 1024) for i in range(2)]
    h2f = self.av(8192, 8 * NT, F32).rearrange("p (c s) -> p c s", c=8); hk = K('h2f')
    wo = []
    for half in range(2):
        wo.append(self.load_wb(l, 6 + half))
    H2 = self.H2S if u.kind == 'lat' else self.H2CS
    AFF = self.AFFS if u.kind == 'lat' else self.AFFCS
    def partA(tq):
        fsb, xk = self.nfs()
        xc = fsb[:, :, 0:NT]
        p.dma('sp', xc, self.xs_ap(u, tq), reads=self.xs_key(u, tq), writes=xk)
        for dc in range(8):
            wb, wk = wo[dc // 4]
            bank = self.nbank()
            for fc in range(8):
                p.op('pe', lambda e, dc=dc, fc=fc, bank=bank, wb=wb, tq=tq: e.matmul(
                    self.ps[bank][:, 0:NT], lhsT=wb[:, fc, (dc % 4) * 128:(dc % 4 + 1) * 128],
                    rhs=self.catT[:, fc, tq * NT:(tq + 1) * NT], start=(fc == 0), stop=(fc == 7)),
                    reads=wk + K('catT', tq), writes=K('ps', bank))
            p.op('dve', lambda e, dc=dc, bank=bank, xc=xc: e.scalar_tensor_tensor(
                out=xc[:, dc, :], in0=self.ps[bank][:, 0:NT], scalar=self.mods[:, 2 * 8 + dc, u.v:u.v + 1],
                in1=xc[:, dc, :], op0=ALU.mult, op1=ALU.add), reads=K('ps', bank) + K('mods') + xk, writes=xk)
        p.dma('sp', self.xs_ap(u, tq), xc, reads=xk, writes=self.xs_key(u, tq))
        return xc, xk
    def partB(tq, xc, xk):
        self.norm_mod(xc, xk, NT, 1, u.v, lambda kc: h2f[:, kc, :], hk)
        for kc in range(8):
            p.op('pe', lambda e, kc=kc: e.matmul(self.ps[4][0:16, 0:NT], lhsT=self.rw[:, l, kc, :], rhs=h2f[:, kc, :],
                                                 start=(kc == 0), stop=(kc == 7)), reads=hk + K('rw'), writes=K('ps', 4))
        ex = self.tmpA[0:16, 0:NT]; rs = self.tmpB[0:16, 0:NT]
        p.op('act', lambda e: e.activation(out=ex, in_=self.ps[4][0:16, 0:NT], func=AF.Exp), reads=K('ps', 4), writes=K('tmpA'))
        p.op('pe', lambda e: e.matmul(self.ps[5][0:16, 0:NT], lhsT=self.onesf[0:16, 0:16], rhs=ex, start=True, stop=True),
             reads=K('tmpA') + K('onesf'), writes=K('ps', 5))
        p.op('dve', lambda e: e.reciprocal(out=rs, in_=self.ps[5][0:16, 0:NT]), reads=K('ps', 5), writes=K('tmpB'))
        p.op('dve', lambda e: e.tensor_tensor(out=ex, in0=ex, in1=rs, op=ALU.mult), reads=K('tmpA') + K('tmpB'), writes=K('tmpA'))
        p.dma('sp', AFF[16 * b:16 * b + 16, tq * NT:(tq + 1) * NT], ex, reads=K('tmpA'), writes=K('AFF' + u.kind))
        p.op('act', lambda e: e.activation(out=h2b[:, 0:4, :], in_=h2f[:, 0:4, :], func=AF.Copy), reads=hk, writes=K('h2b'))
        p.op('dve', lambda e: e.tensor_copy(out=h2b[:, 4:8, :], in_=h2f[:, 4:8, :]), reads=hk, writes=K('h2b'))
        for tt in range(NT // 128):
            for kc in range(8):
                p.op('pe', lambda e, kc=kc, tt=tt: e.transpose(out=self.psb[:, kc * 128:(kc + 1) * 128],
                                                             in_=h2b[:, kc, tt * 128:(tt + 1) * 128], identity=self.identb[:]),
                     reads=K('h2b') + K('identb'), writes=K('psb'))
            hm = h2tm[tt % 2]; hmk = K('h2tm', tt % 2)
            self.evac(hm, self.psb[:, :], K('psb'), hmk)
            p.dma('sp', H2[b, tq * (NT // 128) + tt], hm, reads=hmk, writes=K('H2' + u.kind, b))
    cur = partA(0)
    for tq in range(nq):
        nxt = partA(tq + 1) if tq + 1 < nq else None
        partB(tq, *cur)
        cur = nxt
MK.stage5 = _stage5


def _declare_moe(self):
    nl = self.nl; nb = self.nb
    self.R = 16 * nb
    self.ew1 = self.din("expert_w1", [nl, 16, D, 2 * D])
    self.ew3 = self.din("expert_w3", [nl, 16, D, 2 * D])
    self.ew2 = self.din("expert_w2", [nl, 16, 2 * D, D])
    self.IDXCT = self.dscr("IDXCT", [32, 2, self.R])
    self.MO = [self.dscr("MO%d" % h, [nb * S, 512]) for h in range(2)]
    self.MOC = [self.dscr("MOC%d" % h, [nb * CTX, 512]) for h in range(2)]
    self.out = self.dout("out", [nb, S, D])
    R = self.R
    f0 = self.fs[0].rearrange("p a b -> p (a b)")
    self.GI = f0[:, 0:2 * R].bitcast(U32)
    self.GV = f0[:, 2 * R:4 * R]
    self.GIC = f0[:, 4 * R:4 * R + 16].bitcast(U32)
    self.GVC = f0[:, 4 * R + 16:4 * R + 32]
    self.tmpc = f0[:, 4 * R + 32:4 * R + 64].rearrange("p (w e) -> p w e", w=2)
MK.declare_moe = _declare_moe

def _topk(self, aff_d, L, cap, post):
    p = self.p; R = self.R
    o = 0
    affw = self.bv(o, L, F32)[0:R, :]; o += 2 * L
    vals = self.bv(o, cap, F32)[0:R, :]; o += 2 * cap
    idxu = self.bv(o, cap, U32)[0:R, :]; o += 2 * cap
    idxf = self.bv(o, cap, F32)[0:R, :]; o += 2 * cap
    tT = self.bv(o, 2 * R, F32); o += 4 * R
    p.dma('sp', affw, aff_d, writes=K('affw'))
    for r in range(cap // 8):
        sl = slice(r * 8, (r + 1) * 8)
        p.op('dve', lambda e, sl=sl: e.max(out=vals[:, sl], in_=affw), reads=K('affw'), writes=K('vals'))
        p.op('dve', lambda e, sl=sl: e.max_index(out=idxu[:, sl], in_max=vals[:, sl], in_values=affw),
             reads=K('affw') + K('vals'), writes=K('idxu'))
        p.op('dve', lambda e, sl=sl: e.match_replace(out=affw, in_to_replace=vals[:, sl], in_values=affw, imm_value=-1.0),
             reads=K('affw') + K('vals'), writes=K('affw'))
    p.op('dve', lambda e: e.tensor_copy(out=idxf, in_=idxu), reads=K('idxu'), writes=K('idxf'))
    for cc in range((cap + 127) // 128):
        n = min(128, cap - cc * 128)
        for wi, src in enumerate((idxf, vals)):
            p.op('pe', lambda e, cc=cc, n=n, wi=wi, src=src: e.transpose(
                out=self.ps[wi][0:n, 0:R], in_=src[:, cc * 128:cc * 128 + n], identity=self.identf[0:R, 0:R]),
                reads=K('idxf') + K('vals') + K('cst'), writes=K('ps', wi))
            self.evac(tT[0:n, wi * R:(wi + 1) * R], self.ps[wi][0:n, 0:R], K('ps', wi), K('tT'))
        post(cc, n, tT)
MK.topk = _topk

def _stage6(self, l, do_ctx):
    p = self.p; R = self.R
    def st_lat(cc, n, tT):
        p.op('dve', lambda e: e.tensor_tensor(out=self.GI[:, cc * R:(cc + 1) * R], in0=tT[:, 0:R], in1=self.rowoff[:, 0:R], op=ALU.add),
             reads=K('tT') + K('cst'), writes=K('GI'))
        p.op('dve', lambda e: e.tensor_copy(out=self.GV[:, cc * R:(cc + 1) * R], in_=tT[:, R:2 * R]), reads=K('tT'), writes=K('GV'))
    self.topk(self.AFFS, S, 256, st_lat)
    if do_ctx:
        p.barrier()
        def st_ctx(cc, n, tT):
            p.dma('sp', self.IDXCT, tT[0:32, 0:2 * R].rearrange("p (w r) -> p w r", w=2), reads=K('tT'), writes=K('IDXCT'))
            for b in range(self.nb):
                p.dma('sp', self.tmpc[32 * b:32 * b + 32, :, :], self.IDXCT[:, :, 16 * b:16 * b + 16], reads=K('IDXCT'), writes=K('tmpc'))
            p.op('dve', lambda e: e.tensor_scalar(out=self.GIC[0:32 * self.nb], in0=self.tmpc[0:32 * self.nb, 0, :], scalar1=self.poff[0:32 * self.nb, 0:1],
                                                  scalar2=None, op0=ALU.add), reads=K('tmpc') + K('cst'), writes=K('GIC'))
            p.op('dve', lambda e: e.tensor_copy(out=self.GVC[0:32 * self.nb], in_=self.tmpc[0:32 * self.nb, 1, :]), reads=K('tmpc'), writes=K('GVC'))
        self.topk(self.AFFCS, CTX, 32, st_ctx)
MK.stage6 = _stage6

def _zero_mo(self, do_ctx):
    p = self.p
    z = self.fs[1]
    p.op('dve', lambda e: e.memset(z[:], 0.0), writes=K('fs', 1))
    rows = self.nb * S
    for h in range(2):
        for r0 in range(0, rows, 1024):
            p.dma('sp', self.MO[h][r0:r0 + 1024, :].rearrange("(n p) d -> p n d", p=128), z[:], reads=K('fs', 1), writes=K('MO', h))
        if do_ctx:
            nr = self.nb * CTX
            p.dma('sp', self.MOC[h][0:nr, :].rearrange("(n p) d -> p n d", p=128), z[:, 0:nr // 128, :], reads=K('fs', 1), writes=K('MOC', h))
MK.zero_mo = _zero_mo

def _stage8(self, l, do_ctx):
    p = self.p; nb = self.nb; R = self.R
    nlat = 2 * nb
    nch = nlat + (1 if do_ctx else 0)
    NS = nch * 128
    ncs = 32 * nb
    o = 0
    xeT = self.bv(o, 8 * NS).rearrange("p (k n) -> p k n", k=8); o += 8 * 1152
    xg = self.bv(o, 9 * 1024).rearrange("p (c d) -> p c d", c=9); o += 9 * 1024
    gT = self.bv(o, 16 * NS).rearrange("p (f n) -> p f n", f=16); o += 16 * 1152
    w2h = [self.bv(o + i * 8192, 8192).rearrange("p (f d) -> p f d", f=16) for i in range(2)]; o += 16384
    wbm = [self.bv(o + i * 4096, 4096).rearrange("p (k c) -> p k c", k=8) for i in range(4)]; o += 16384
    assert o <= 32768 + NARENA, o
    ysc = self.sqb
    splits = [(n0, min(512, NS - n0)) for n0 in range(0, NS, 512)] if NS % 384 else [(n0, 384) for n0 in range(0, NS, 384)]
    H2f = self.H2S.rearrange("b t p d -> (b t p) d")
    H2cf = self.H2CS.rearrange("b t p d -> (b t p) d")
    if do_ctx and ncs < 128:
        p.op('dve', lambda e: e.memset(xg[:, nlat, :], 0.0), writes=K('xg', nlat))
    wi = 0
    def gathers(e_):
        for ch in range(nch):
            if ch < nlat:
                b, cc = ch // 2, ch % 2
                col = self.GI[:, cc * R + 16 * b + e_:cc * R + 16 * b + e_ + 1]
                p.dma('pool', None, None, fn=lambda e, ch=ch, col=col: e.indirect_dma_start(
                    xg[:, ch, :], None, H2f, bass.IndirectOffsetOnAxis(ap=col, axis=0)), reads=K('GI'), writes=K('xg', ch))
            else:
                col = self.GIC[0:ncs, e_:e_ + 1]
                p.dma('pool', None, None, fn=lambda e, ch=ch, col=col: e.indirect_dma_start(
                    xg[0:ncs, ch, :], None, H2cf, bass.IndirectOffsetOnAxis(ap=col, axis=0)), reads=K('GIC'), writes=K('xg', ch))
    def w13(e_, Fg):
        nonlocal wi
        wa = wbm[wi % 4]; wak = K('wbm', wi % 4); wi += 1
        wu = wbm[wi % 4]; wuk = K('wbm', wi % 4); wi += 1
        p.dma('pool', wa, self.ew1[l, e_, :, Fg * 512:(Fg + 1) * 512].rearrange("(k p) c -> p k c", p=128), writes=wak)
        p.dma('pool', wu, self.ew3[l, e_, :, Fg * 512:(Fg + 1) * 512].rearrange("(k p) c -> p k c", p=128), writes=wuk)
        return wa, wak, wu, wuk
    def w2load(e_, dh):
        p.dma('pool', w2h[dh], self.ew2[l, e_, :, dh * 512:(dh + 1) * 512].rearrange("(f p) d -> p f d", p=128), writes=K('w2h', dh))
    gathers(0)
    pre13 = {0: [w13(0, 0), w13(0, 1)]}
    w2load(0, 0); w2load(0, 1)
    for e_ in range(16):
        for ch in range(nch):
            for kc in range(8):
                p.op('pe', lambda e, ch=ch, kc=kc: e.transpose(out=self.psb[:, kc * 128:(kc + 1) * 128],
                                                             in_=xg[:, ch, kc * 128:(kc + 1) * 128], identity=self.identb[:]),
                     reads=K('xg', ch) + K('identb'), writes=K('psb'))
            self.evac(xeT[:, :, ch * 128:(ch + 1) * 128], self.psb[:, :].rearrange("p (k s) -> p k s", k=8), K('psb'), K('xeT'))
        for Fg in range(4):
            if Fg < 2:
                wa, wak, wu, wuk = pre13[e_][Fg]
            else:
                wa, wak, wu, wuk = w13(e_, Fg)
            for fl in range(4):
                fcn = Fg * 4 + fl
                for si, (n0, nn) in enumerate(splits):
                    bA = self.nbank(); bU = self.nbank()
                    for kc in range(8):
                        p.op('pe', lambda e, kc=kc, fl=fl, n0=n0, nn=nn, bA=bA, wa=wa: e.matmul(
                            self.ps[bA][:, 0:nn], lhsT=wa[:, kc, fl * 128:(fl + 1) * 128], rhs=xeT[:, kc, n0:n0 + nn],
                            start=(kc == 0), stop=(kc == 7)), reads=wak + K('xeT'), writes=K('ps', bA))
                    for kc in range(8):
                        p.op('pe', lambda e, kc=kc, fl=fl, n0=n0, nn=nn, bU=bU, wu=wu: e.matmul(
                            self.ps[bU][:, 0:nn], lhsT=wu[:, kc, fl * 128:(fl + 1) * 128], rhs=xeT[:, kc, n0:n0 + nn],
                            start=(kc == 0), stop=(kc == 7)), reads=wuk + K('xeT'), writes=K('ps', bU))
                    sg = self.ntb[si % 2][:, 0:nn]; sgk = K('ntb', si % 2)
                    p.op('act', lambda e, sg=sg, bA=bA, nn=nn: e.activation(out=sg, in_=self.ps[bA][:, 0:nn], func=AF.Silu),
                         reads=K('ps', bA), writes=sgk)
                    p.op('dve', lambda e, sg=sg, bU=bU, nn=nn, fcn=fcn, n0=n0: e.tensor_tensor(
                        out=gT[:, fcn, n0:n0 + nn], in0=self.ps[bU][:, 0:nn], in1=sg, op=ALU.mult),
                        reads=K('ps', bU) + sgk, writes=K('gT'))
        if e_ + 1 < 16:
            gathers(e_ + 1)
            pre13[e_ + 1] = [w13(e_ + 1, 0), w13(e_ + 1, 1)]
        yi = 0
        for dh in range(2):
            for ch in range(nch):
                bank = self.nbank()
                for fc in range(16):
                    p.op('pe', lambda e, fc=fc, ch=ch, dh=dh, bank=bank: e.matmul(
                        self.ps[bank], lhsT=gT[:, fc, ch * 128:(ch + 1) * 128], rhs=w2h[dh][:, fc, :],
                        start=(fc == 0), stop=(fc == 15)), reads=K('gT') + K('w2h', dh), writes=K('ps', bank))
                ys = ysc[yi % 2]; ysk = K('sqb', yi % 2); yi += 1
                if ch < nlat:
                    b, cc = ch // 2, ch % 2
                    gcol = self.GV[:, cc * R + 16 * b + e_:cc * R + 16 * b + e_ + 1]; gk = K('GV')
                    icol = self.GI[:, cc * R + 16 * b + e_:cc * R + 16 * b + e_ + 1]; ik = K('GI')
                    dst = self.MO[dh]; dk = K('MO', dh, b); npart = 128
                else:
                    gcol = self.GVC[0:ncs, e_:e_ + 1]; gk = K('GVC')
                    icol = self.GIC[0:ncs, e_:e_ + 1]; ik = K('GIC')
                    dst = self.MOC[dh]; dk = K('MOC', dh); npart = ncs
                eng = 'act' if yi % 2 else 'dve'
                if eng == 'act':
                    p.op('act', lambda e, ys=ys, bank=bank, gcol=gcol, npart=npart: e.activation(
                        out=ys[0:npart], in_=self.ps[bank][0:npart], func=AF.Identity, scale=gcol), reads=K('ps', bank) + gk, writes=ysk)
                else:
                    p.op('dve', lambda e, ys=ys, bank=bank, gcol=gcol, npart=npart: e.tensor_scalar(
                        out=ys[0:npart], in0=self.ps[bank][0:npart], scalar1=gcol, scalar2=None, op0=ALU.mult),
                        reads=K('ps', bank) + gk, writes=ysk)
                p.dma('pool', None, None, fn=lambda e, ys=ys, dst=dst, icol=icol, npart=npart: e.indirect_dma_start(
                    dst, bass.IndirectOffsetOnAxis(ap=icol, axis=0), ys[0:npart], None, compute_op=ALU.add),
                    reads=ysk + ik, writes=dk)
            if e_ + 1 < 16:
                w2load(e_ + 1, dh)
MK.stage8 = _stage8

def _scatter_setup(self, u):
    p = self.p; b = u.b; NT = u.NT
    MOs = self.MO if u.kind == 'lat' else self.MOC
    row0 = b * u.L
    def pre(tq, xc, xk):
        for tt in range(NT // 128):
            tok0 = row0 + tq * NT + tt * 128
            xt = self.xtm[:, tt % 2, :]; xtk = K('xtm', tt % 2)
            for h in range(2):
                p.dma('sp', xt[:, h * 512:(h + 1) * 512], MOs[h][tok0:tok0 + 128, :], reads=K('MO' + u.kind), writes=xtk)
            for half in range(2):
                bank = self.nbank()
                for j in range(4):
                    kc = half * 4 + j
                    p.op('pe', lambda e, kc=kc, j=j, bank=bank, xt=xt: e.transpose(
                        out=self.ps[bank][:, j * 128:(j + 1) * 128], in_=xt[:, kc * 128:(kc + 1) * 128],
                        identity=self.identf), reads=xtk + K('cst'), writes=K('ps', bank))
                for j in range(4):
                    kc = half * 4 + j
                    p.op('dve', lambda e, kc=kc, j=j, bank=bank, tt=tt: e.scalar_tensor_tensor(
                        out=xc[:, kc, tt * 128:(tt + 1) * 128], in0=self.ps[bank][:, j * 128:(j + 1) * 128],
                        scalar=self.g2p[:, kc, u.v:u.v + 1], in1=xc[:, kc, tt * 128:(tt + 1) * 128], op0=ALU.mult, op1=ALU.add),
                        reads=K('ps', bank) + K('g2p') + xk, writes=xk)
    return pre
MK.scatter_setup = _scatter_setup

def _final_unit(self, u, pre):
    p = self.p; NT = u.NT
    otm = [self.bv(0 + i * 2048, 1024, F32) for i in range(2)]
    oi = 0
    for tq in range(u.nq):
        fsb, xk = self.nfs()
        xc = fsb[:, :, 0:NT]
        p.dma('sp', xc, self.xs_ap(u, tq), reads=self.xs_key(u, tq), writes=xk)
        pre(tq, xc, xk)
        f2, ok = self.nfs()
        of = f2[:, :, 0:NT]
        self.norm_mod(xc, xk, NT, 2, u.v, lambda kc: of[:, kc, :], ok)
        for tt in range(NT // 128):
            ot = otm[oi % 2]; otk = K('otm', oi % 2); oi += 1
            for half in range(2):
                bank = self.nbank()
                for j in range(4):
                    kc = half * 4 + j
                    p.op('pe', lambda e, kc=kc, j=j, tt=tt, bank=bank, of=of: e.transpose(
                        out=self.ps[bank][:, j * 128:(j + 1) * 128], in_=of[:, kc, tt * 128:(tt + 1) * 128],
                        identity=self.identf), reads=ok + K('cst'), writes=K('ps', bank))
                self.evac(ot[:, half * 512:(half + 1) * 512], self.ps[bank], K('ps', bank), otk)
            tok0 = tq * NT + tt * 128
            p.dma('sp', self.out[u.b, tok0:tok0 + 128, :], ot, reads=otk, writes=K('out', u.b, tq, tt))
MK.final_unit = _final_unit

def _build(self, sched):
    p = self.p; nb = self.nb; nl = self.nl
    self.declare(); self.declare_hyena(); self.declare_cf(); self.declare_moe()
    if getattr(self, 'dbg', None): self.dbg('decl', 0, None)
    self.init_consts()
    lat = [Unit('lat', b, nb) for b in range(nb)]
    ctxu = [Unit('ctx', b, nb) for b in range(nb)]
    for l in range(nl):
        last = (l == nl - 1)
        p.barrier()
        if l > 0:
            p.op('dve', lambda e: e.tensor_copy(out=self.g2p[:], in_=self.mods[:, 40:48, :]), reads=K('mods'), writes=K('g2p'))
        p.stage = 'adaln'
        self.convert_weights(l)
        self.adaln(l)
        p.stage = 'filter'
        self.hyena_filter(l, S); p.barrier()
        if not last:
            self.hyena_filter(l, CTX); p.barrier()
        for b in range(nb):
            for u in (ctxu[b], lat[b]):
                pre = None
                if l > 0:
                    p.stage = 'scatter'
                    pre = self.scatter_setup(u)
                if pre is None: p.stage = 'stage1'
                self.stage1(l, u, pre); p.barrier(True)
                p.stage = 'attn'
                if getattr(self, 'dbg', None): self.dbg('s1', l, u)
                self.attn_stage(l, u, sched, last); p.barrier(True)
                if u.kind == 'ctx' and last:
                    continue
                p.stage = 'hyena'
                self.hyena_unit(l, u); p.barrier(True)
                p.stage = 'conformer'
                self.conformer_unit(l, u); p.barrier(True)
                p.stage = 'stage5'
                self.stage5(l, u); p.barrier(True)
                if getattr(self, 'dbg', None): self.dbg('s5', l, u)
        p.stage = 'topk'
        p.barrier()
        self.zero_mo(do_ctx=not last)
        self.stage6(l, do_ctx=not last); p.barrier()
        p.stage = 'experts'
        self.stage8(l, do_ctx=not last); p.barrier()
    p.op('dve', lambda e: e.tensor_copy(out=self.g2p[:], in_=self.mods[:, 40:48, :]), reads=K('mods'), writes=K('g2p'))
    p.stage = 'final'
    for b in range(nb):
        pre = self.scatter_setup(lat[b])
        self.final_unit(lat[b], pre); p.barrier()
    p.finish()
MK.build = _build


GRID_W = 64; ROWS = 32; WIN_H = 8; WIN_W = 16

def na_schedule():
    r0 = lambda r: min(max(r - WIN_H // 2, 0), ROWS - WIN_H)
    specs = {}; sched = []
    for i in range(16):
        lst = []
        for j in range(16):
            dr = [[(2 * j + a) - (2 * i + b) for b in range(2)] for a in range(2)]
            iw = [[int(r0(2 * i + b) <= 2 * j + a < r0(2 * i + b) + WIN_H) for b in range(2)] for a in range(2)]
            if not any(iw[a][b] for a in range(2) for b in range(2)):
                continue
            key = (tuple(map(tuple, dr)), tuple(map(tuple, iw)))
            if key not in specs:
                specs[key] = len(specs)
            lst.append((j, specs[key]))
        sched.append(lst)
    keys = list(specs.keys())
    order = [t for (_, t) in sched[7]]
    order += [t for t in range(len(keys)) if t not in order]
    remap = {old: new for new, old in enumerate(order)}
    sched = [[(j, remap[t]) for (j, t) in lst] for lst in sched]
    return sched, [keys[old] for old in order]

def na_bias_table(rpb):
    sched, specs = na_schedule()
    H = rpb.shape[0]; NT = len(specs)
    col = np.arange(GRID_W)
    c0 = np.clip(col - WIN_W // 2, 0, GRID_W - WIN_W)
    col_ok = (col[None, :] >= c0[:, None]) & (col[None, :] < c0[:, None] + WIN_W)
    dc = np.clip(col[None, :] - col[:, None], 1 - WIN_W, WIN_W - 1) + WIN_W - 1
    tab = np.full((2, 64, H, NT, 2, 64), -1e30, dtype=np.float32)
    for t, (dr, iw) in enumerate(specs):
        for a in range(2):
            for b in range(2):
                if not iw[a][b]:
                    continue
                dri = dr[a][b] + WIN_H - 1
                vals = rpb[:, dri, :][:, dc]
                vals = np.where(col_ok[None], vals, np.float32(-1e30))
                tab[a, :, :, t, b, :] = vals.transpose(2, 0, 1)
    return np.ascontiguousarray(tab.reshape(128, H, NT, 128))

def base_inputs(inp, b0, nb):
    c = inp["c"][b0:b0 + nb]; c_ctx = inp["c_ctx"]
    cv = np.concatenate([c, c_ctx[None]], 0)
    cT = np.ascontiguousarray(cv.reshape(nb + 1, 8, 128).transpose(2, 1, 0))
    nl = inp["w_mod"].shape[0]
    b_modT = np.ascontiguousarray(inp["b_mod"].reshape(nl, 48, 128).transpose(2, 0, 1))
    ng = np.stack([inp["norm1_g"][l] for l in range(nl)] + [inp["norm2_g"][l] for l in range(nl)] + [inp["final_norm_g"]], 0)
    ngT = np.ascontiguousarray(ng.reshape(2 * nl + 1, 8, 128).transpose(2, 0, 1))
    cst = np.zeros((128, 128 + 512 + 16 + 64 + 1), np.float32)
    cst[:, 0:128] = np.eye(128)
    cst[:, 128:640] = np.arange(512)[None, :]
    cst[:, 640:656] = np.arange(128)[:, None] + 128 * np.arange(16)[None, :]
    cst[:, 656:720] = 2048 * (np.arange(64) // 16)[None, :]
    cst[:, 720] = 256 * (np.arange(128) // 32)
    m = {
        "x": np.ascontiguousarray(inp["x"][b0:b0 + nb]),
        "ctx": np.ascontiguousarray(inp["ctx"][b0:b0 + nb]),
        "cT": cT, "w_mod": inp["w_mod"], "b_modT": b_modT, "ngT": ngT, "w_in": inp["w_in"], "w_out": inp["w_out"],
        "cst_f": cst, "ident_b": np.eye(128).astype(BF),
        "na_tab": np.stack([na_bias_table(inp["na_rpb"][l]) for l in range(nl)]),
    }
    return m

import math
def dft_consts(L):
    N = 2 * L
    nC = L // 128; NT = min(512, L); nq = L // NT; FG = min(4, nC); nfg = nC // FG
    t = np.arange(L, dtype=np.float64); f = np.arange(L, dtype=np.float64) + 0.5
    ang = 2 * np.pi * np.outer(t, f) / N
    C = np.cos(ang); Sn = np.sin(ang)
    A = np.stack([C, Sn], 0).reshape(2, nC, 128, nC, 128)
    A = np.ascontiguousarray(A.transpose(3, 2, 0, 1, 4)).astype(BF)
    Bm = np.stack([C.T, Sn.T], 0).reshape(2, nfg, FG, 128, nq, NT)
    Bm = np.ascontiguousarray(Bm.transpose(4, 1, 3, 0, 2, 5)).astype(BF)
    return A, Bm

def hyena_pos_consts(L):
    f32 = np.float32
    pos = np.arange(L, dtype=f32)[:, None]
    t = pos / f32(max(L - 1, 1))
    bands = 16
    fb = np.linspace(1e-4, bands - 1, bands, dtype=f32)[None, :]
    ang = fb * f32(2.0 * math.pi) * pos / f32(L)
    z = np.concatenate([t, np.cos(ang), -np.sin(ang)], axis=-1).astype(f32)
    zT = np.zeros((64, L), f32); zT[:33] = z.T
    min_decay = math.log(1e-2) / 1.5; max_decay = math.log(1e-2) / 0.3
    rate = np.abs(np.linspace(min_decay, max_decay, 256, dtype=f32))
    dec = np.exp(-t * rate).astype(f32)
    decb = dec.copy(); decb[0] = 0.0
    dd = np.stack([dec, decb], 1).reshape(L // 128, 128, 2, 256).transpose(1, 0, 2, 3)
    return zT, np.ascontiguousarray(dd)

def hyena_inputs(inp):
    nl = inp["hy_filt_w1"].shape[0]
    m = {}
    for L in (2048, 256):
        A, Bm = dft_consts(L)
        zT, dd = hyena_pos_consts(L)
        m["dftA%d" % L] = A; m["dftB%d" % L] = Bm; m["zpos%d" % L] = zT; m["decay%d" % L] = dd
    hyw = np.zeros((nl, 64, 64 + 64 + 1024 + 4), np.float32)
    hyw[:, :33, 0:64] = inp["hy_filt_w1"]
    hyw[:, :, 64:128] = inp["hy_filt_w2"]
    hyw[:, :, 128:1152] = inp["hy_filt_w3"]
    hyw[:, :, 1152] = inp["hy_filt_b1"]; hyw[:, :, 1153] = inp["hy_filt_freq"]; hyw[:, :, 1154] = inp["hy_filt_b2"]
    m["hyw"] = hyw
    sw = inp["hy_short_w"].reshape(nl, 3, 6, 128).transpose(0, 3, 2, 1)
    sb = inp["hy_short_b"].reshape(nl, 6, 128).transpose(0, 2, 1)[..., None]
    dd = inp["hy_bias_d"].reshape(nl, 2, 2, 128).transpose(0, 3, 1, 2)
    m["hysc"] = np.ascontiguousarray(np.concatenate([sw.reshape(nl, 128, 18), sb.reshape(nl, 128, 6), dd.reshape(nl, 128, 4)], -1).transpose(1, 0, 2))
    return m

def cf_moe_inputs(inp):
    nl = inp["cf_dw_w"].shape[0]
    m = {}
    cw = inp["cf_dw_w"].reshape(nl, 31, 2, 128).transpose(3, 0, 2, 1)
    oth = np.stack([inp["cf_dw_b"], inp["cf_ln_g"], inp["cf_ln_b"]], 1).reshape(nl, 3, 2, 128).transpose(3, 0, 2, 1)
    m["cfsc"] = np.ascontiguousarray(np.concatenate([cw, oth], -1))
    m["router_wT"] = np.ascontiguousarray(inp["router_w"].reshape(nl, 8, 128, 16).transpose(2, 0, 1, 3))
    m["expert_w1"] = inp["expert_w1"]; m["expert_w3"] = inp["expert_w3"]; m["expert_w2"] = inp["expert_w2"]
    return m


NCORES = 8

def kernel(**inputs):
    from concourse.bass_utils import run_bass_kernel_spmd
    inp = {k: np.asarray(v) for k, v in inputs.items()}
    B = inp["x"].shape[0]
    nb = B // NCORES
    sched, _ = na_schedule()
    mk = MK(nb=nb, nl=NL)
    mk.build(sched)
    shared = {}
    shared.update(hyena_inputs(inp))
    shared.update(cf_moe_inputs(inp))
    in_maps = []
    for c in range(NCORES):
        m = base_inputs(inp, c * nb, nb)
        m.update(shared)
        in_maps.append({k: v for k, v in m.items() if k in mk.dram})
    res = run_bass_kernel_spmd(mk.nc, in_maps, core_ids=list(range(NCORES)))
    out = np.concatenate([np.asarray(r["out"]) for r in res.results], axis=0)
    return out.astype(np.float32, copy=False)
```

```python
import numpy as np
from contextlib import ExitStack
import concourse.bass as bass
import concourse.mybir as mybir

F32 = mybir.dt.float32
BF16 = mybir.dt.bfloat16
U32 = mybir.dt.uint32
I32 = mybir.dt.int32
AF = mybir.ActivationFunctionType
ALU = mybir.AluOpType
AX = mybir.AxisListType

ENGS = ['pe', 'act', 'dve', 'pool', 'sp']
STRICT = {'pe': False, 'act': True, 'dve': True, 'pool': True, 'sp': False}
NDMASEM = 12


class _Call:
    def __init__(self, name, args, kwargs):
        self.name = name; self.args = args; self.kwargs = kwargs
    def __call__(self, engine):
        return getattr(engine, self.name)(*self.args, **self.kwargs)


class _Proxy:
    def __getattr__(self, name):
        def rec(*args, **kwargs):
            return _Call(name, args, kwargs)
        return rec

_PROXY = _Proxy()


class Prog:
    def __init__(self, nc):
        self.nc = nc
        self.es = ExitStack()
        self.streams = {e: [] for e in ENGS}
        self.cnt = {}
        self.seen = {e: {} for e in ENGS}
        self.lastw = {}
        self.readers = {}
        self.sems = {}
        for e in ENGS:
            self.sems[e] = self.es.enter_context(nc.semaphore("s_" + e))
            self.cnt[e] = 0
        self.dq_n = {}
        for q in ['sp', 'pool', 'act']:
            self.dq_n[q] = 0
            for i in range(NDMASEM):
                k = ('dq', q, i)
                self.sems[k] = self.es.enter_context(nc.semaphore("d_%s%d" % (q, i)))
                self.cnt[k] = 0
        self.nops = 0
        self.marked = {e: set() for e in ENGS}

    def sbuf(self, name, shape, dtype):
        return self.es.enter_context(self.nc.sbuf_tensor(name, list(shape), dtype))

    def psum(self, name, shape, dtype=F32):
        return self.es.enter_context(self.nc.psum_tensor(name, list(shape), dtype))

    def _deps(self, eng, reads, writes):
        need = {}
        def add(tok, raw):
            k, v = tok
            if k == eng and not raw:
                return
            if need.get(k, 0) < v:
                need[k] = v
        for b in reads:
            if b in self.lastw:
                add(self.lastw[b], True)
        for b in writes:
            if b in self.lastw:
                add(self.lastw[b], False)
            for t in self.readers.get(b, ()):
                add(t, False)
        st = self.streams[eng]
        for k, v in need.items():
            if k == eng and not STRICT[eng]:
                continue
            if self.seen[eng].get(k, 0) >= v:
                continue
            st.append(('wait', k, v))
            self.seen[eng][k] = v
            if k in self.marked:
                self.marked[k].add(v)

    def _commit(self, tok, reads, writes):
        for b in writes:
            self.lastw[b] = tok
            self.readers[b] = []
        for b in reads:
            self.readers.setdefault(b, []).append(tok)

    def op(self, eng, fn, reads=(), writes=()):
        reads = list(reads); writes = list(writes)
        self._deps(eng, reads, writes)
        self.cnt[eng] += 1
        fn = fn(_PROXY)
        assert isinstance(fn, _Call)
        self.streams[eng].append(('op', fn, eng, self.cnt[eng], getattr(self, 'stage', '')))
        self._commit((eng, self.cnt[eng]), reads, writes)
        self.nops += 1

    def dma(self, q, out, in_, reads=(), writes=(), fn=None, **kw):
        reads = list(reads); writes = list(writes)
        self._deps(q, reads, writes)
        i = self.dq_n[q] % NDMASEM
        self.dq_n[q] += 1
        k = ('dq', q, i)
        if self.cnt[k] > 0 and self.seen[q].get(k, 0) < self.cnt[k]:
            self.streams[q].append(('wait', k, self.cnt[k]))
            self.seen[q][k] = self.cnt[k]
        self.cnt[k] += 16
        if fn is None:
            fn = (lambda e, out=out, in_=in_, kw=kw: e.dma_start(out=out, in_=in_, **kw))
        else:
            fn = fn(_PROXY)
        self.streams[q].append(('op', fn, k, 16))
        self._commit((k, self.cnt[k]), reads, writes)
        self.nops += 1

    def finish(self):
        st = self.streams['sp']
        for k, v in self.cnt.items():
            if v > 0 and k != 'sp' and self.seen['sp'].get(k, 0) < v:
                st.append(('wait', k, v))
                if k in self.marked:
                    self.marked[k].add(v)
        rank = {}
        for e in ENGS:
            rank[e] = {v: i + 1 for i, v in enumerate(sorted(self.marked[e]))}
        self.rank = rank
        nc = self.nc
        emap = {'pe': 'tensor', 'act': 'scalar', 'dve': 'vector', 'pool': 'gpsimd', 'sp': 'sync'}
        with nc.Block() as block:
            for e in ENGS:
                stream = self.streams[e]
                if not stream:
                    continue
                def body(engine, stream=stream):
                    for it in stream:
                        if it[0] == 'wait':
                            k, v = it[1], it[2]
                            if k in self.rank:
                                v = self.rank[k][v]
                            engine.wait_ge(self.sems[k], v)
                        else:
                            ins = it[1](engine)
                            k = it[2]
                            if k in self.rank:
                                if it[3] in self.rank[k]:
                                    ins.then_inc(self.sems[k], 1)
                            else:
                                ins.then_inc(self.sems[k], it[3])
                getattr(block, emap[e])(body)
        self.es.close()


def K(name, *idx):
    out = [(name,)]
    for ix in idx:
        if isinstance(ix, (list, tuple, range)):
            out = [o + (i,) for o in out for i in ix]
        else:
            out = [o + (ix,) for o in out]
    return out


def _barrier(self, skip_pool=False):
    for e in ENGS:
        if skip_pool and e == 'pool':
            continue
        for k, v in self.cnt.items():
            if skip_pool and (k == 'pool' or (isinstance(k, tuple) and k[1] == 'pool')):
                continue
            if v > 0 and k != e and self.seen[e].get(k, 0) < v:
                self.streams[e].append(('wait', k, v))
                self.seen[e][k] = v
                if k in self.marked:
                    self.marked[k].add(v)
Prog.barrier = _barrier


import numpy as np
import ml_dtypes

D = 1024; S = 2048; CTX = 256; DIN = 2816; NL = 2
EPS = 1e-6
BF = ml_dtypes.bfloat16
NARENA = 36896
NSLOT = 1152


class Unit:
    def __init__(self, kind, b, nb):
        self.kind = kind; self.b = b
        self.L = S if kind == 'lat' else CTX
        self.NT = min(512, self.L); self.nq = self.L // self.NT
        self.v = b if kind == 'lat' else nb
        self.cap = self.L // 8


class MK:
    def __init__(self, nb=4, nl=2):
        self.nb = nb; self.nl = nl
        self.nv = nb + 1
        self.nc = nc = bass.Bass("TRN2", target_bir_lowering=False)
        self.p = Prog(nc)
        self.dram = {}
        self.evi = 0; self.pbank = 0; self.wbi = 0; self.fsi = 0

    def din(self, name, shape, dtype=F32):
        self.dram[name] = self.nc.dram_tensor(name, list(shape), dtype, kind="ExternalInput").ap()
        return self.dram[name]
    def dout(self, name, shape, dtype=F32):
        self.dram[name] = self.nc.dram_tensor(name, list(shape), dtype, kind="ExternalOutput").ap()
        return self.dram[name]
    def dscr(self, name, shape, dtype=F32):
        self.dram[name] = self.nc.dram_tensor(name, list(shape), dtype).ap()
        return self.dram[name]

    def bv(self, off, n, dtype=BF16):
        if dtype == BF16:
            return self.big[:, off:off + n]
        assert off % 2 == 0
        return self.big[:, off:off + 2 * n].bitcast(dtype)

    def av(self, off, n, dtype=BF16):
        if dtype == BF16:
            return self.arena[:, off:off + n]
        assert off % 2 == 0
        return self.arena[:, off:off + 2 * n].bitcast(dtype)

    def declare(self):
        nb, nl, nv = self.nb, self.nl, self.nv
        p = self.p
        self.x = self.din("x", [nb, S, D])
        self.ctx = self.din("ctx", [nb, CTX, D])
        self.cT = self.din("cT", [128, 8, nv])
        self.w_mod = self.din("w_mod", [nl, D, 6 * D])
        self.b_modT = self.din("b_modT", [128, nl, 48])
        self.ngT = self.din("ngT", [128, 2 * nl + 1, 8])
        self.w_in = self.din("w_in", [nl, D, DIN])
        self.w_out = self.din("w_out", [nl, D, D])
        self.cst_f = self.din("cst_f", [128, 128 + 512 + 16 + 64 + 1])
        self.ident_b = self.din("ident_b", [128, 128], BF16)
        self.na_tab = self.din("na_tab", [nl, 128, 8, 9, 128])
        self.XS = self.dscr("XS", [nb, 4, 128, 8, 512])
        self.XCS = self.dscr("XCS", [nb, 1, 128, 8, 256])
        self.WB = self.dscr("WB", [nl, 8, 128, 8, 512], BF16)
        self.cst = p.sbuf("cst", [128, 128 + 512 + 16 + 64 + 1], F32)
        self.identf = self.cst[:, 0:128]
        self.iota512 = self.cst[:, 128:640]
        self.tokid = self.cst[:, 640:656]
        self.rowoff = self.cst[:, 656:720]
        self.poff = self.cst[:, 720:721]
        self.onesb = p.sbuf("onesb", [128, 128], BF16)
        self.identb = p.sbuf("identb", [128, 128], BF16)
        self.onesf = p.sbuf("onesf", [128, 128], F32)
        self.cTs = p.sbuf("cTs", [128, 8, nv], F32)
        self.mods = p.sbuf("mods", [128, 48, nv], F32)
        self.G = p.sbuf("G", [128, 2, 8, nv], F32)
        self.bmod = p.sbuf("bmod", [128, nl, 48], F32)
        self.ng = p.sbuf("ng", [128, 2 * nl + 1, 8], F32)
        self.epsc = p.sbuf("epsc", [128, 1], F32)
        self.g2p = p.sbuf("g2p", [128, 8, nv], F32)
        self.psall = p.psum("psall", [128, 7 * 512], F32)
        self.ps = [self.psall[:, i * 512:(i + 1) * 512] for i in range(7)]
        self.psb = p.psum("psb", [128, 1024], BF16)
        self.NW = 2
        self.wb = [p.sbuf("wb%d" % i, [128, 8, 512], BF16) for i in range(self.NW)]
        self.fs = [p.sbuf("fs%d" % i, [128, 8, 512], F32) for i in range(2)]
        self.sqb = [p.sbuf("sqb%d" % i, [128, 512], F32) for i in range(2)]
        self.ntb = [p.sbuf("ntb%d" % i, [128, 512], F32) for i in range(2)]
        self.tmpA = p.sbuf("tmpA", [128, 512], F32)
        self.tmpB = p.sbuf("tmpB", [128, 512], F32)
        self.kcT = p.sbuf("kcT", [128, 4, CTX], BF16)
        self.vc = p.sbuf("vc", [128, 2, 8, 65], BF16)
        self.big = p.sbuf("big", [128, 32768 + NARENA], BF16)
        self.hT = self.big[:, 0:16384].rearrange("p (c s) -> p c s", c=8)
        self.catT = self.big[:, 16384:32768].rearrange("p (c s) -> p c s", c=8)
        self.arena = self.big[:, 32768:32768 + NARENA]
        self.xtm = self.av(0, 2048, F32).rearrange("p (a b) -> p a b", a=2)
        a = 0
        self.qT = self.av(a, 8192).rearrange("p (c s) -> p c s", c=4); a += 8192
        self.kT = self.av(a, 8192).rearrange("p (c s) -> p c s", c=4); a += 8192
        self.v = self.av(a, 8320).rearrange("p (t h d) -> p t h d", t=16, h=8); a += 8320
        self.EB = self.av(a, 9216).rearrange("p (h t q) -> p h t q", h=8, t=9); a += 9216
        self.PT = [self.av(a + i * 896, 896) for i in range(2)]; a += 1792
        self.attn_tm = self.av(a, 512); a += 512
        self.rinv = self.av(a, 8, F32); a += 16
        assert a <= NARENA

    def init_consts(self):
        p = self.p
        p.dma('sp', self.cst[:], self.cst_f, writes=K('cst'))
        p.dma('sp', self.identb[:], self.ident_b, writes=K('identb'))
        p.dma('sp', self.cTs[:], self.cT, writes=K('cTs'))
        p.dma('sp', self.bmod[:], self.b_modT, writes=K('bmod'))
        p.dma('sp', self.ng[:], self.ngT, writes=K('ng'))
        p.op('dve', lambda e: e.memset(self.onesf[:], 1.0), writes=K('onesf'))
        p.op('dve', lambda e: e.memset(self.onesb[:], 1.0), writes=K('onesb'))
        p.op('dve', lambda e: e.memset(self.epsc[:], EPS), writes=K('epsc'))
        p.op('dve', lambda e: e.memset(self.vc[:], 1.0), writes=K('vc'))
        p.op('act', lambda e: e.activation(out=self.cTs[:], in_=self.cTs[:], func=AF.Silu),
             reads=K('cTs'), writes=K('cTs'))

    def evac(self, out, in_, reads, writes, eng=None):
        p = self.p
        if eng is None:
            eng = 'act' if (self.evi % 2) else 'dve'
            self.evi += 1
        if eng == 'act':
            p.op('act', lambda e: e.activation(out=out, in_=in_, func=AF.Copy), reads=reads, writes=writes)
        else:
            p.op('dve', lambda e: e.tensor_copy(out=out, in_=in_), reads=reads, writes=writes)

    def load_w(self, src):
        i = self.wbi; self.wbi = (self.wbi + 1) % self.NW
        a, b = src.shape[1], src.shape[2]
        dst = self.wb[i].rearrange("p a b -> p (a b)")[:, 0:a * b].rearrange("p (a b) -> p a b", a=a)
        self.p.dma('pool', dst, src, writes=K('wb', i))
        return dst, K('wb', i)

    WPIECE = {0: 0, 512: 1, 1024: 2, 1536: 3, 2048: 4, 2304: 5}

    def load_wb(self, l, idx, ncols=512):
        i = self.wbi; self.wbi = (self.wbi + 1) % self.NW
        dst = self.wb[i][:, :, 0:ncols]
        self.p.dma('pool', dst, self.WB[l, idx][:, :, 0:ncols], reads=K('WB', l, idx), writes=K('wb', i))
        return dst, K('wb', i)

    def convert_weights(self, l):
        p = self.p
        for c0, idx in self.WPIECE.items():
            nc_ = min(512, DIN - c0)
            wbuf, wk = self.load_w(self.w_in[l, :, c0:c0 + nc_].rearrange("(kc p) c -> p kc c", p=128))
            p.dma('sp', self.WB[l, idx][:, :, 0:nc_], wbuf, reads=wk, writes=K('WB', l, idx))
        for half in range(2):
            wbuf, wk = self.load_w(self.w_out[l, :, half * 512:(half + 1) * 512].rearrange("(kc p) c -> p kc c", p=128))
            p.dma('sp', self.WB[l, 6 + half], wbuf, reads=wk, writes=K('WB', l, 6 + half))

    def nbank(self):
        b = self.pbank; self.pbank = (self.pbank + 1) % 4
        return b

    def nfs(self):
        i = self.fsi; self.fsi ^= 1
        return self.fs[i], K('fs', i)

    def adaln(self, l):
        p = self.p; nv = self.nv
        for piece in range(12):
            st, sk = self.nfs()
            src = self.w_mod[l, :, piece * 512:(piece + 1) * 512].rearrange("(kc p) c -> p kc c", p=128)
            p.dma('sp', st[:], src, writes=sk)
            ps = self.ps[5]
            for j in range(4):
                for kc in range(8):
                    p.op('pe', lambda e, j=j, kc=kc, st=st: e.matmul(
                        ps[:, j * 8:j * 8 + nv], lhsT=st[:, kc, j * 128:(j + 1) * 128], rhs=self.cTs[:, kc, :],
                        start=(kc == 0), stop=(kc == 7)),
                        reads=sk + K('cTs'), writes=K('ps', 5))
            for j in range(4):
                oc = piece * 4 + j
                p.op('dve', lambda e, j=j, oc=oc: e.tensor_scalar(
                    out=self.mods[:, oc, :], in0=ps[:, j * 8:j * 8 + nv], scalar1=self.bmod[:, l, oc:oc + 1],
                    scalar2=None, op0=ALU.add), reads=K('ps', 5) + K('bmod'), writes=K('mods'))
        for which, m in ((0, 1), (1, 4)):
            gi = l if which == 0 else self.nl + l
            for kc in range(8):
                p.op('dve', lambda e, which=which, m=m, kc=kc, gi=gi: e.tensor_scalar(
                    out=self.G[:, which, kc, :], in0=self.mods[:, m * 8 + kc, :], scalar1=1.0,
                    scalar2=self.ng[:, gi, kc:kc + 1], op0=ALU.add, op1=ALU.mult),
                    reads=K('mods') + K('ng'), writes=K('G'))

    def norm_mod(self, xc, xkeys, n, which, v, outf, outkeys):
        p = self.p
        ps = self.ps[6]
        for kc in range(8):
            sq = self.sqb[kc % 2].bitcast(BF16); sqk = K('sqb', kc % 2)
            p.op('act', lambda e, kc=kc, sq=sq: e.activation(out=sq[:, 0:n], in_=xc[:, kc, :], func=AF.Square),
                 reads=xkeys, writes=sqk)
            p.op('pe', lambda e, kc=kc, sq=sq: e.matmul(ps[:, 0:n], lhsT=self.onesb[:], rhs=sq[:, 0:n],
                                                        start=(kc == 0), stop=(kc == 7)),
                 reads=sqk + K('onesb'), writes=K('ps', 6))
        p.op('act', lambda e: e.activation(out=self.tmpA[:, 0:n], in_=ps[:, 0:n], func=AF.Sqrt,
                                           bias=self.epsc[:, 0:1], scale=1.0 / D),
             reads=K('ps', 6) + K('epsc'), writes=K('tmpA'))
        p.op('dve', lambda e: e.reciprocal(out=self.tmpB[:, 0:n], in_=self.tmpA[:, 0:n]),
             reads=K('tmpA'), writes=K('tmpB'))
        for kc in range(8):
            nt = self.ntb[kc % 2]; ntk = K('ntb', kc % 2)
            p.op('dve', lambda e, kc=kc, nt=nt: e.tensor_tensor(out=nt[:, 0:n], in0=xc[:, kc, :], in1=self.tmpB[:, 0:n],
                                                               op=ALU.mult),
                 reads=xkeys + K('tmpB'), writes=ntk)
            if which == 2:
                sc = self.ng[:, 2 * self.nl, kc:kc + 1]; bi = 0.0
            else:
                sc = self.G[:, which, kc, v:v + 1]
                bi = self.mods[:, (0 if which == 0 else 3) * 8 + kc, v:v + 1]
            p.op('act', lambda e, kc=kc, nt=nt, sc=sc, bi=bi: e.activation(out=outf(kc), in_=nt[:, 0:n], func=AF.Identity,
                                                                           scale=sc, bias=bi),
                 reads=ntk + K('G') + K('mods') + K('ng'), writes=outkeys)

    def xs_ap(self, u, tq):
        return (self.XS if u.kind == 'lat' else self.XCS)[u.b, tq][:, :, 0:u.NT]
    def xs_key(self, u, tq):
        return K('XS' + u.kind, u.b, tq)

    def load_x_input(self, u, tq, xc, xk):
        p = self.p
        src = self.x if u.kind == 'lat' else self.ctx
        for tt in range(u.NT // 128):
            tok0 = tq * u.NT + tt * 128
            xt = self.xtm[:, tt % 2, :]; xtk = K('xtm', tt % 2)
            p.dma('sp', xt, src[u.b, tok0:tok0 + 128, :], writes=xtk)
            for half in range(2):
                bank = self.nbank()
                for j in range(4):
                    kc = half * 4 + j
                    p.op('pe', lambda e, kc=kc, j=j, bank=bank, xt=xt: e.transpose(
                        out=self.ps[bank][:, j * 128:(j + 1) * 128], in_=xt[:, kc * 128:(kc + 1) * 128],
                        identity=self.identf), reads=xtk + K('cst'), writes=K('ps', bank))
                self.evac(xc[:, half * 4:half * 4 + 4, tt * 128:(tt + 1) * 128],
                          self.ps[bank].rearrange("p (j t) -> p j t", j=4), K('ps', bank), xk)

    def stage1(self, l, u, pre=None):
        p = self.p
        def fetch(tq):
            fsb, xk = self.nfs()
            xc = fsb[:, :, 0:u.NT]
            if l == 0:
                self.load_x_input(u, tq, xc, xk)
                p.dma('sp', self.xs_ap(u, tq), xc, reads=xk, writes=self.xs_key(u, tq))
            else:
                p.dma('sp', xc, self.xs_ap(u, tq), reads=self.xs_key(u, tq), writes=xk)
                if pre is not None:
                    pre(tq, xc, xk)
                    p.dma('sp', self.xs_ap(u, tq), xc, reads=xk, writes=self.xs_key(u, tq))
            return xc, xk
        cur = fetch(0)
        for tq in range(u.nq):
            nxt = fetch(tq + 1) if tq + 1 < u.nq else None
            xc, xk = cur
            self.norm_mod(xc, xk, u.NT, 0, u.v,
                          lambda kc, tq=tq: self.hT[:, kc, tq * u.NT:(tq + 1) * u.NT], K('hT', tq))
            cur = nxt

    def proj_fm(self, l, col0, nblk, u, evac):
        p = self.p
        NT = u.NT
        for pc in range(0, nblk, 4):
            nb_ = min(4, nblk - pc)
            c0 = col0 + pc * 128
            wsrc = self.w_in[l, :, c0:c0 + nb_ * 128].rearrange("(kc p) c -> p kc c", p=128)
            wb, wk = self.load_w(wsrc)
            for ml in range(nb_):
                for tq in range(u.nq):
                    bank = self.nbank()
                    for kc in range(8):
                        p.op('pe', lambda e, kc=kc, ml=ml, tq=tq, bank=bank, wb=wb: e.matmul(
                            self.ps[bank][:, 0:NT], lhsT=wb[:, kc, ml * 128:(ml + 1) * 128],
                            rhs=self.hT[:, kc, tq * NT:(tq + 1) * NT], start=(kc == 0), stop=(kc == 7)),
                            reads=wk + K('hT', tq), writes=K('ps', bank))
                    evac(pc + ml, tq, self.ps[bank][:, 0:NT], K('ps', bank))

    def proj_v(self, l, u, vdst, vkeys):
        p = self.p
        wsrc = self.w_in[l, :, 1024:1536].rearrange("(kc p) c -> p kc c", p=128)
        wb, wk = self.load_w(wsrc)
        for tt in range(u.L // 128):
            bank = self.nbank()
            for kc in range(8):
                p.op('pe', lambda e, kc=kc, tt=tt, bank=bank: e.matmul(
                    self.ps[bank], lhsT=self.hT[:, kc, tt * 128:(tt + 1) * 128], rhs=wb[:, kc, :],
                    start=(kc == 0), stop=(kc == 7)),
                    reads=wk + K('hT', (tt * 128) // u.NT), writes=K('ps', bank))
            self.evac(vdst[:, tt, :, 0:64], self.ps[bank].rearrange("p (h d) -> p h d", d=64),
                      K('ps', bank), vkeys(tt))

    def load_na_tables(self, l):
        p = self.p
        for h in range(8):
            st, sk = self.nfs()
            stv = st.rearrange("p a b -> p (a b)")[:, 0:9 * 128]
            p.dma('sp', stv, self.na_tab[l, :, h].rearrange("p t q -> p (t q)"), writes=sk)
            p.op('act', lambda e, h=h, stv=stv: e.activation(
                out=self.EB[:, h].rearrange("p t q -> p (t q)"), in_=stv, func=AF.Copy, scale=8.0),
                reads=sk, writes=K('EB'))

    def attention(self, qT, qkeys, blocks, kT, v, out_catT):
        p = self.p
        items = [(bi, h) for bi in range(len(blocks)) for h in range(8)]
        def qk(k):
            bi, h = items[k]
            q0, local = blocks[bi]
            nloc = len(local); ntot = nloc + 2
            hp, r0 = h // 2, (h % 2) * 64
            sb = 2 + 2 * (k % 2)
            ST = self.psall[:, sb * 512: sb * 512 + 1024]
            stk = K('ps', sb) + K('ps', sb + 1)
            for n in range(ntot):
                if n < nloc:
                    j = local[n][0]
                    lhsT = kT[r0:r0 + 64, hp, j * 128:(j + 1) * 128]; lk = K('kT', j // 4)
                else:
                    jc = n - nloc
                    lhsT = self.kcT[r0:r0 + 64, hp, jc * 128:(jc + 1) * 128]; lk = K('kcT')
                p.op('pe', lambda e, n=n, lhsT=lhsT: e.matmul(
                    ST[:, n * 128:(n + 1) * 128], lhsT=lhsT, rhs=qT[r0:r0 + 64, hp, q0:q0 + 128],
                    start=(n % 4 == 0), stop=False), reads=lk + qkeys(q0), writes=[stk[n // 4]])
            tids = [t for (_, t) in local]
            runs = []
            for n, t in enumerate(tids):
                if runs and runs[-1][1] + runs[-1][2] == t and (n // 4) == (runs[-1][0] // 4):
                    runs[-1][2] += 1
                else:
                    runs.append([n, t, 1])
            for n0_, t0_, ln in runs:
                p.op('pe', lambda e, n0_=n0_, t0_=t0_, ln=ln: e.matmul(
                    ST[:, n0_ * 128:(n0_ + ln) * 128], lhsT=self.identb[:],
                    rhs=self.EB[:, h, t0_:t0_ + ln, :].rearrange("p t q -> p (t q)"), start=False, stop=True),
                    reads=K('EB') + K('identb'), writes=[stk[n0_ // 4]])
        qk(0)
        pending = None
        for k, (bi, h) in enumerate(items):
            if k + 1 < len(items):
                qk(k + 1)
            q0, local = blocks[bi]
            nloc = len(local); ntot = nloc + 2
            sb = 2 + 2 * (k % 2); pb = k % 2
            ST = self.psall[:, sb * 512: sb * 512 + 1024]
            stk = K('ps', sb) + K('ps', sb + 1)
            pt = self.PT[pb]
            p.op('act', lambda e, ST=ST, pt=pt, ntot=ntot: e.activation(
                out=pt[:, 0:ntot * 128], in_=ST[:, 0:ntot * 128], func=AF.Exp, scale=0.125),
                reads=stk, writes=K('PT', pb, range(ntot)))
            ob = h // 4; oc0 = (h % 4) * 65
            for n in range(ntot):
                if n < nloc:
                    j = local[n][0]; rhs = v[:, j, h, :]; rk = K('v', j)
                else:
                    jc = n - nloc; rhs = self.vc[:, jc, h, :]; rk = K('vc')
                p.op('pe', lambda e, n=n, rhs=rhs, pt=pt, ob=ob, oc0=oc0, ntot=ntot: e.matmul(
                    self.ps[ob][:, oc0:oc0 + 65], lhsT=pt[:, n * 128:(n + 1) * 128], rhs=rhs,
                    start=(n == 0), stop=(n == ntot - 1)), reads=K('PT', pb, n) + rk, writes=K('ps', ob))
            if pending is not None and h == 0:
                pending(); pending = None
            if h == 7:
                for ob in range(2):
                    p.op('dve', lambda e, ob=ob: e.reciprocal(
                        out=self.rinv[:, ob * 4:(ob + 1) * 4],
                        in_=self.ps[ob][:, 0:260].rearrange("p (h d) -> p h d", d=65)[:, :, 64]),
                        reads=K('ps', ob), writes=K('rinv', ob))
                for hh in range(8):
                    ob = hh // 4; oc0 = (hh % 4) * 65
                    p.op('dve', lambda e, hh=hh, ob=ob, oc0=oc0: e.tensor_scalar(
                        out=self.attn_tm[:, hh * 64:(hh + 1) * 64], in0=self.ps[ob][:, oc0:oc0 + 64],
                        scalar1=self.rinv[:, hh:hh + 1], scalar2=None, op0=ALU.mult),
                        reads=K('ps', ob) + K('rinv', ob), writes=K('attn_tm', hh // 2))
                def fin(bi=bi):
                    for c4 in range(4):
                        p.op('pe', lambda e, c4=c4: e.transpose(out=self.psb[:, c4 * 128:(c4 + 1) * 128],
                                                               in_=self.attn_tm[:, c4 * 128:(c4 + 1) * 128], identity=self.identb[:]),
                             reads=K('attn_tm', c4) + K('identb'), writes=K('psb'))
                    out_ap, out_keys = out_catT(bi)
                    self.evac(out_ap, self.psb[:, 0:512].rearrange("p (c q) -> p c q", c=4), K('psb'), out_keys, eng='dve')
                pending = fin
        if pending is not None:
            pending()

    def attn_stage(self, l, u, sched, last):
        p = self.p
        NT = u.NT
        if u.kind == 'ctx':
            self.proj_fm(l, 512, 4, u, lambda m, tq, ps, pk: self.evac(self.kcT[:, m, :], ps, pk, K('kcT')))
            self.proj_v(l, u, self.vc, lambda tt: K('vc'))
            if last:
                return
            self.proj_fm(l, 0, 4, u, lambda m, tq, ps, pk: self.evac(self.qT[:, m, 0:CTX], ps, pk, K('qT', 0)))
            blocks = [(i * 128, []) for i in range(2)]
            self.attention(self.qT, lambda q0: K('qT', 0), blocks, None, None,
                           lambda bi: (self.catT[:, 0:4, bi * 128:(bi + 1) * 128], K('catT', 0)))
        else:
            p.op('dve', lambda e: e.memset(self.v[:, :, :, 64:65], 1.0), writes=K('v', range(16)))
            self.load_na_tables(l)
            self.proj_fm(l, 0, 4, u, lambda m, tq, ps, pk: self.evac(self.qT[:, m, tq * NT:(tq + 1) * NT], ps, pk, K('qT', tq)))
            self.proj_fm(l, 512, 4, u, lambda m, tq, ps, pk: self.evac(self.kT[:, m, tq * NT:(tq + 1) * NT], ps, pk, K('kT', tq)))
            self.proj_v(l, u, self.v, lambda tt: K('v', tt))
            blocks = [(i * 128, sched[i]) for i in range(16)]
            self.attention(self.qT, lambda q0: K('qT', q0 // 512), blocks, self.kT, self.v,
                           lambda bi: (self.catT[:, 0:4, bi * 128:(bi + 1) * 128], K('catT', bi // 4)))


TWO_PI = 2.0 * np.pi

def _declare_hyena(self):
    nl = self.nl
    self.dftA = {}; self.dftB = {}; self.zpos = {}; self.decay = {}; self.KS = {}
    for L in (2048, 256):
        nC = L // 128; NT = min(512, L); nq = L // NT; FG = min(4, nC); nfg = nC // FG
        self.dftA[L] = self.din("dftA%d" % L, [nC, 128, 2, nC, 128], BF16)
        self.dftB[L] = self.din("dftB%d" % L, [nq, nfg, 128, 2, FG, NT], BF16)
        self.zpos[L] = self.din("zpos%d" % L, [64, L])
        self.decay[L] = self.din("decay%d" % L, [128, nC, 2, 256])
        self.KS[L] = self.dscr("KS%d" % L, [nC, 128, 2, 512])
    self.hyw_d = self.din("hyw", [nl, 64, 1156])
    self.hysc_d = self.din("hysc", [128, nl, 28])

    self.hysc = self.p.sbuf("hysc_s", [128, nl, 28], F32)
    self.p.dma('sp', self.hysc[:], self.hysc_d, writes=K('hysc'))
MK.declare_hyena = _declare_hyena

def _sin_layer(self, src_ps, n, bcol, fcol, dst):
    p = self.p
    A = self.tmpA[0:64, 0:n]; Bt = self.tmpB[0:64, 0:n]
    p.op('dve', lambda e: e.tensor_scalar(out=A, in0=src_ps, scalar1=bcol, scalar2=fcol, op0=ALU.add, op1=ALU.mult),
         reads=K('ps', 4) + K('hyw'), writes=K('tmpA'))
    p.op('dve', lambda e: e.tensor_scalar(out=Bt, in0=A, scalar1=float(np.pi), scalar2=-TWO_PI, op0=ALU.is_gt, op1=ALU.mult),
         reads=K('tmpA'), writes=K('tmpB'))
    p.op('dve', lambda e: e.tensor_tensor(out=A, in0=A, in1=Bt, op=ALU.add), reads=K('tmpA') + K('tmpB'), writes=K('tmpA'))
    p.op('dve', lambda e: e.tensor_scalar(out=Bt, in0=A, scalar1=-float(np.pi), scalar2=TWO_PI, op0=ALU.is_lt, op1=ALU.mult),
         reads=K('tmpA'), writes=K('tmpB'))
    p.op('dve', lambda e: e.tensor_tensor(out=A, in0=A, in1=Bt, op=ALU.add), reads=K('tmpA') + K('tmpB'), writes=K('tmpA'))
    p.op('act', lambda e: e.activation(out=dst, in_=A, func=AF.Sin), reads=K('tmpA'), writes=K('fs', 0) + K('fs', 1))
MK.sin_layer = _sin_layer

def _hyena_filter(self, l, L):
    p = self.p
    nC = L // 128; NT = min(512, L); nq = L // NT
    fs0 = self.fs[0].rearrange("p a b -> p (a b)"); fs1 = self.fs[1].rearrange("p a b -> p (a b)")
    zp = fs0[0:64, 0:L]; h1 = fs0[0:64, L:2 * L]; h2 = fs1[0:64, 0:L]
    kp = self.av(0, 8192).rearrange("p (t n) -> p t n", t=16)
    km = self.av(8192, 8192).rearrange("p (t n) -> p t n", t=16)
    hyw = self.av(16384, 1156, F32)[0:64, :]
    dcb = [self.av(16384 + 2320 + i * 1024, 512, F32).rearrange("p (a b) -> p a b", a=2) for i in range(2)]
    abuf = [self.av(16384 + 2320 + 2048 + i * 4096, 4096).rearrange("p (c t f) -> p c t f", c=2, t=16) for i in range(2)]
    p.dma('sp', zp, self.zpos[L], writes=K('fs', 0))
    p.dma('sp', hyw, self.hyw_d[l], writes=K('hyw'))
    w1 = hyw[:, 0:64]; w2 = hyw[:, 64:128]; w3 = hyw[:, 128:1152]
    for tq in range(nq):
        ps = self.ps[4][0:64, 0:NT]
        p.op('pe', lambda e, tq=tq: e.matmul(ps, lhsT=w1, rhs=zp[:, tq * NT:(tq + 1) * NT], start=True, stop=True),
             reads=K('hyw') + K('fs', 0), writes=K('ps', 4))
        self.sin_layer(ps, NT, hyw[:, 1152:1153], hyw[:, 1153:1154], h1[:, tq * NT:(tq + 1) * NT])
    for tq in range(nq):
        ps = self.ps[4][0:64, 0:NT]
        p.op('pe', lambda e, tq=tq: e.matmul(ps, lhsT=w2, rhs=h1[:, tq * NT:(tq + 1) * NT], start=True, stop=True),
             reads=K('hyw') + K('fs', 0), writes=K('ps', 4))
        self.sin_layer(ps, NT, hyw[:, 1154:1155], hyw[:, 1153:1154], h2[:, tq * NT:(tq + 1) * NT])
    for tc in range(nC):
        dc = dcb[tc % 2]; dk = K('dcb', tc % 2)
        p.dma('sp', dc, self.decay[L][:, tc], writes=dk)
        for half in range(2):
            p.op('pe', lambda e, tc=tc, half=half: e.matmul(
                self.ps[half], lhsT=h2[:, tc * 128:(tc + 1) * 128], rhs=w3[:, half * 512:(half + 1) * 512],
                start=True, stop=True), reads=K('hyw') + K('fs', 1), writes=K('ps', half))
        fw = self.ntb[0]; bw = self.ntb[1]
        for o in range(2):
            p.op('dve', lambda e, o=o, dc=dc: e.tensor_tensor(out=fw[:, o * 256:(o + 1) * 256], in0=self.ps[0][:, o * 256:(o + 1) * 256],
                                                            in1=dc[:, 0, :], op=ALU.mult), reads=K('ps', 0) + dk, writes=K('ntb', 0))
            p.op('dve', lambda e, o=o, dc=dc: e.tensor_tensor(out=bw[:, o * 256:(o + 1) * 256], in0=self.ps[1][:, o * 256:(o + 1) * 256],
                                                            in1=dc[:, 1, :], op=ALU.mult), reads=K('ps', 1) + dk, writes=K('ntb', 1))
        p.op('dve', lambda e, tc=tc: e.tensor_tensor(out=kp[:, tc, :], in0=fw[:], in1=bw[:], op=ALU.add),
             reads=K('ntb', 0) + K('ntb', 1), writes=K('kp'))
        p.op('dve', lambda e, tc=tc: e.tensor_tensor(out=km[:, tc, :], in0=fw[:], in1=bw[:], op=ALU.subtract),
             reads=K('ntb', 0) + K('ntb', 1), writes=K('km'))
    for fc in range(nC):
        ab = abuf[fc % 2][:, :, 0:nC, :]; ak = K('abuf', fc % 2)
        p.dma('sp', ab, self.dftA[L][fc], writes=ak)
        b0 = 2 * (fc % 2)
        for tc in range(nC):
            p.op('pe', lambda e, tc=tc, ab=ab, b0=b0: e.matmul(self.ps[b0], lhsT=ab[:, 0, tc, :], rhs=kp[:, tc, :],
                                                            start=(tc == 0), stop=(tc == nC - 1)),
                 reads=ak + K('kp'), writes=K('ps', b0))
            p.op('pe', lambda e, tc=tc, ab=ab, b0=b0: e.matmul(self.ps[b0 + 1], lhsT=ab[:, 1, tc, :], rhs=km[:, tc, :],
                                                            start=(tc == 0), stop=(tc == nC - 1)),
                 reads=ak + K('km'), writes=K('ps', b0 + 1))
        for cs in range(2):
            self.evac(self.sqb[cs][:], self.ps[b0 + cs], K('ps', b0 + cs), K('sqb', cs))
            p.dma('sp', self.KS[L][fc, :, cs, :], self.sqb[cs][:], reads=K('sqb', cs), writes=K('KS', L, fc))
MK.hyena_filter = _hyena_filter

def _hyena_unit(self, l, u):
    p = self.p
    L = u.L; NT = u.NT; nq = u.nq; nC = L // 128; FG = min(4, nC); nfg = nC // FG
    a = 0
    xg = [self.av(a + i * 4096, 4096).rearrange("p (c s) -> p c s", c=2) for i in range(3)]; a += 12288
    ub = [self.av(a, 2064) for i in range(2)]; a += 2064
    ztok = self.av(a, 4096).rearrange("p (t n) -> p t n", t=16); a += 4096
    Yre = self.av(a, 4096).rearrange("p (f n) -> p f n", f=16); a += 4096
    Ys = self.av(a, 4096).rearrange("p (f n) -> p f n", f=16); a += 4096
    fsb_ = [self.fs[i].rearrange("p a b -> p (a b)").bitcast(BF16) for i in range(2)]
    slot = [fsb_[s // 2][:, (s % 2) * 4096:(s % 2 + 1) * 4096] for s in range(4)]
    ksb = [self.av(a + i * 2048, 1024, F32).rearrange("p (c n) -> p c n", c=2) for i in range(2)]; a += 4096
    assert a <= NARENA
    fs0 = self.fs[0].rearrange("p a b -> p (a b)"); fs1 = self.fs[1].rearrange("p a b -> p (a b)")
    sc = self.hysc
    for i in range(2):
        p.op('dve', lambda e, i=i: e.memset(ub[i][:, 0:1], 0.0), writes=K('ub', i))
        p.op('dve', lambda e, i=i: e.memset(ub[i][:, L + 1:L + 2], 0.0), writes=K('ub', i))
    def ev(m, tq, ps, pk):
        ubm = ub[0]; uk = K('ub', 0)
        self.evac(ubm[:, 1 + tq * NT:1 + (tq + 1) * NT], ps, pk, uk)
        if tq == nq - 1:
            dest = xg[m // 2][:, m % 2, 0:L]; dk = K('xg', m // 2)
            T1 = fs0[:, 0:L]; T2 = fs1[:, 0:L]
            p.op('dve', lambda e: e.tensor_scalar(out=T1, in0=ubm[:, 1:L + 1], scalar1=sc[:, l, m * 3 + 1:m * 3 + 2],
                                                  scalar2=sc[:, l, 18 + m:19 + m], op0=ALU.mult, op1=ALU.add),
                 reads=uk + K('hysc'), writes=K('fs', 0))
            p.op('dve', lambda e: e.scalar_tensor_tensor(out=T2, in0=ubm[:, 0:L], scalar=sc[:, l, m * 3:m * 3 + 1], in1=T1,
                                                         op0=ALU.mult, op1=ALU.add),
                 reads=uk + K('hysc') + K('fs', 0), writes=K('fs', 1))
            p.op('dve', lambda e: e.scalar_tensor_tensor(out=dest, in0=ubm[:, 2:L + 2], scalar=sc[:, l, m * 3 + 2:m * 3 + 3], in1=T2,
                                                         op0=ALU.mult, op1=ALU.add),
                 reads=uk + K('hysc') + K('fs', 1), writes=dk)
    self.proj_fm(l, 1536, 6, u, ev)
    p.barrier(True)
    z = xg[2]
    for o in range(2):
        gate = xg[o]
        for t4 in range(0, nC, 4):
            nt4 = min(4, nC - t4)
            for tl in range(nt4):
                for cch in range(2):
                    p.op('pe', lambda e, tl=tl, cch=cch, t4=t4: e.transpose(
                        out=self.psb[:, tl * 256 + cch * 128: tl * 256 + (cch + 1) * 128],
                        in_=z[:, cch, (t4 + tl) * 128:(t4 + tl + 1) * 128], identity=self.identb[:]),
                        reads=K('xg', 2) + K('identb'), writes=K('psb'))
            self.evac(ztok[:, t4:t4 + nt4, :], self.psb[:, 0:nt4 * 256].rearrange("p (t n) -> p t n", t=nt4),
                      K('psb'), K('ztok'))
        for fp in range(nC // 2):
            ak = K('fsh', 2 * (fp % 2)) + K('fsh', 2 * (fp % 2) + 1)
            abq = [slot[2 * (fp % 2) + q][:, 0:2 * nC * 128].rearrange("p (c t f) -> p c t f", c=2, t=nC) for q in range(2)]
            for q in range(2):
                p.dma('sp', abq[q], self.dftA[L][2 * fp + q], writes=K('fsh', 2 * (fp % 2) + q))
            ks = ksb[fp % 2]; kk = K('ksb', fp % 2)
            for q in range(2):
                p.dma('sp', ks[:, :, q * 256:(q + 1) * 256], self.KS[L][2 * fp + q, :, :, o * 256:(o + 1) * 256],
                      reads=K('KS', L, 2 * fp + q), writes=kk)
            b0 = 2 * (fp % 2)
            for q in range(2):
                for tc in range(nC):
                    p.op('pe', lambda e, tc=tc, q=q: e.matmul(self.ps[b0][:, q * 256:(q + 1) * 256], lhsT=abq[q][:, 0, tc, :], rhs=ztok[:, tc, :],
                                                             start=(tc == 0), stop=(tc == nC - 1)),
                         reads=ak + K('ztok'), writes=K('ps', b0))
                for tc in range(nC):
                    p.op('pe', lambda e, tc=tc, q=q: e.matmul(self.ps[b0 + 1][:, q * 256:(q + 1) * 256], lhsT=abq[q][:, 1, tc, :], rhs=ztok[:, tc, :],
                                                             start=(tc == 0), stop=(tc == nC - 1)),
                         reads=ak + K('ztok'), writes=K('ps', b0 + 1))
            Zr = self.ps[b0]; Zs = self.ps[b0 + 1]
            Kr = ks[:, 0, :]; Ksn = ks[:, 1, :]
            n0 = self.ntb[0]; n1 = self.ntb[1]; n2 = self.sqb[0]; n3 = self.sqb[1]
            yre = Yre[:, 2 * fp:2 * fp + 2, :].rearrange("p q n -> p (q n)")
            ysv = Ys[:, 2 * fp:2 * fp + 2, :].rearrange("p q n -> p (q n)")
            p.op('dve', lambda e: e.tensor_tensor(out=n0[:], in0=Zr, in1=Kr, op=ALU.mult), reads=K('ps', b0) + kk, writes=K('ntb', 0))
            p.op('dve', lambda e: e.tensor_tensor(out=n1[:], in0=Zs, in1=Ksn, op=ALU.mult), reads=K('ps', b0 + 1) + kk, writes=K('ntb', 1))
            p.op('dve', lambda e: e.tensor_tensor(out=yre, in0=n0[:], in1=n1[:], op=ALU.subtract),
                 reads=K('ntb', 0) + K('ntb', 1), writes=K('Yre'))
            p.op('dve', lambda e: e.tensor_tensor(out=n2[:], in0=Zr, in1=Ksn, op=ALU.mult), reads=K('ps', b0) + kk, writes=K('sqb', 0))
            p.op('dve', lambda e: e.tensor_tensor(out=n3[:], in0=Zs, in1=Kr, op=ALU.mult), reads=K('ps', b0 + 1) + kk, writes=K('sqb', 1))
            p.op('dve', lambda e: e.tensor_tensor(out=ysv, in0=n2[:], in1=n3[:], op=ALU.add),
                 reads=K('sqb', 0) + K('sqb', 1), writes=K('Ys'))
        bi = 0
        for tq in range(nq):
            b0 = 2 * (tq % 2)
            for fg in range(nfg):
                bb = slot[bi % 4][:, 0:2 * FG * NT].rearrange("p (c f t) -> p c f t", c=2, f=FG); bk = K('fsh', bi % 4); bi += 1
                p.dma('sp', bb, self.dftB[L][tq, fg], writes=bk)
                for fl in range(FG):
                    fc = fg * FG + fl
                    for cch in range(2):
                        p.op('pe', lambda e, fc=fc, fl=fl, cch=cch, bb=bb, b0=b0: e.matmul(
                            self.ps[b0 + cch][:, 0:NT], lhsT=Yre[:, fc, cch * 128:(cch + 1) * 128], rhs=bb[:, 0, fl, :],
                            start=(fc == 0), stop=False), reads=bk + K('Yre'), writes=K('ps', b0 + cch))
                        p.op('pe', lambda e, fc=fc, fl=fl, cch=cch, bb=bb, b0=b0: e.matmul(
                            self.ps[b0 + cch][:, 0:NT], lhsT=Ys[:, fc, cch * 128:(cch + 1) * 128], rhs=bb[:, 1, fl, :],
                            start=False, stop=(fc == nC - 1)), reads=bk + K('Ys'), writes=K('ps', b0 + cch))
            for cch in range(2):
                zc = z[:, cch, tq * NT:(tq + 1) * NT]
                n0 = self.ntb[cch][:, 0:NT]; nk = K('ntb', cch)
                n1 = self.sqb[cch][:, 0:NT]; sk = K('sqb', cch)
                dcol = sc[:, l, 24 + o * 2 + cch:25 + o * 2 + cch]
                p.op('dve', lambda e, zc=zc, n0=n0, dcol=dcol: e.tensor_scalar(out=n0, in0=zc, scalar1=dcol, scalar2=None, op0=ALU.mult),
                     reads=K('xg', 2) + K('hysc'), writes=nk)
                p.op('dve', lambda e, n0=n0, n1=n1, b0=b0, cch=cch: e.scalar_tensor_tensor(
                    out=n1, in0=self.ps[b0 + cch][:, 0:NT], scalar=1.0 / L, in1=n0, op0=ALU.mult, op1=ALU.add),
                    reads=K('ps', b0 + cch) + nk, writes=sk)
                if o == 0:
                    dst = zc; dk = K('xg', 2)
                else:
                    dst = self.catT[:, 4 + cch, tq * NT:(tq + 1) * NT]; dk = K('catT', tq)
                p.op('dve', lambda e, n1=n1, dst=dst, cch=cch, tq=tq, gate=gate: e.tensor_tensor(
                    out=dst, in0=n1, in1=gate[:, cch, tq * NT:(tq + 1) * NT], op=ALU.mult),
                    reads=sk + K('xg', o), writes=dk)
MK.hyena_unit = _hyena_unit


def _declare_cf(self):
    nl = self.nl; p = self.p; nb = self.nb
    self.cfsc_d = self.din("cfsc", [128, nl, 2, 34])
    self.cfsc = p.sbuf("cfsc_s", [128, nl, 2, 34], F32)
    p.dma('sp', self.cfsc[:], self.cfsc_d, writes=K('cfsc'))
    self.rw_d = self.din("router_wT", [128, nl, 8, 16])
    self.rw = p.sbuf("rw_s", [128, nl, 8, 16], F32)
    p.dma('sp', self.rw[:], self.rw_d, writes=K('rw'))
    self.H2S = self.dscr("H2S", [nb, 16, 128, 1024], BF16)
    self.H2CS = self.dscr("H2CS", [nb, 2, 128, 1024], BF16)
    self.AFFS = self.dscr("AFFS", [16 * nb, S])
    self.AFFCS = self.dscr("AFFCS", [16 * nb, CTX])
MK.declare_cf = _declare_cf

def _conformer_unit(self, l, u):
    p = self.p
    L = u.L; NT = u.NT; nq = u.nq
    LP = L + 30
    a = 0
    glu = self.av(a, 2 * LP).rearrange("p (c s) -> p c s", c=2); a += 2 * LP + (2 * LP) % 2
    diag = self.av(a, 7936).rearrange("p (c j f) -> p c j f", c=2, j=31); a += 7936
    atmp = self.av(a, 2 * L).rearrange("p (c s) -> p c s", c=2); a += 2 * L
    assert a <= NARENA
    cs = self.cfsc
    p.op('dve', lambda e: e.memset(glu[:, :, 0:15], 0.0), writes=K('glu'))
    p.op('dve', lambda e: e.memset(glu[:, :, L + 15:L + 30], 0.0), writes=K('glu'))
    for cch in range(2):
        for j in range(31):
            p.op('dve', lambda e, cch=cch, j=j: e.tensor_scalar(out=diag[:, cch, j, :], in0=self.identf,
                                                              scalar1=cs[:, l, cch, j:j + 1], scalar2=None, op0=ALU.mult),
                 reads=K('cst') + K('cfsc'), writes=K('diag'))
    def ev(m, tq, ps, pk):
        if m < 2:
            self.evac(atmp[:, m, tq * NT:(tq + 1) * NT], ps, pk, K('atmp'))
        else:
            cch = m - 2
            sg = self.ntb[cch][:, 0:NT]
            p.op('act', lambda e: e.activation(out=sg, in_=ps, func=AF.Sigmoid), reads=pk, writes=K('ntb', cch))
            p.op('dve', lambda e: e.tensor_tensor(out=glu[:, cch, 15 + tq * NT:15 + (tq + 1) * NT], in0=sg,
                                                  in1=atmp[:, cch, tq * NT:(tq + 1) * NT], op=ALU.mult),
                 reads=K('ntb', cch) + K('atmp'), writes=K('glu'))
    self.proj_fm(l, 2304, 4, u, ev)
    for tq in range(nq):
        yb = self.fs[0]; ysq = self.fs[1]
        for cch in range(2):
            bank = self.nbank()
            for j in range(31):
                p.op('pe', lambda e, cch=cch, j=j, bank=bank, tq=tq: e.matmul(
                    self.ps[bank][:, 0:NT], lhsT=diag[:, cch, j, :], rhs=glu[:, cch, tq * NT + j:tq * NT + j + NT],
                    start=(j == 0), stop=(j == 30)), reads=K('diag') + K('glu'), writes=K('ps', bank))
            p.op('dve', lambda e, cch=cch, bank=bank: e.tensor_scalar(out=yb[:, cch, 0:NT], in0=self.ps[bank][:, 0:NT],
                                                                    scalar1=cs[:, l, cch, 31:32], scalar2=None, op0=ALU.add),
                 reads=K('ps', bank) + K('cfsc'), writes=K('fs', 0))
            p.op('act', lambda e, cch=cch: e.activation(out=ysq[:, cch, 0:NT], in_=yb[:, cch, 0:NT], func=AF.Square),
                 reads=K('fs', 0), writes=K('fs', 1))
        for cch in range(2):
            p.op('pe', lambda e, cch=cch: e.matmul(self.ps[4][:, 0:NT], lhsT=self.onesf[:], rhs=yb[:, cch, 0:NT],
                                                  start=(cch == 0), stop=(cch == 1)), reads=K('fs', 0) + K('onesf'), writes=K('ps', 4))
        for cch in range(2):
            p.op('pe', lambda e, cch=cch: e.matmul(self.ps[5][:, 0:NT], lhsT=self.onesf[:], rhs=ysq[:, cch, 0:NT],
                                                  start=(cch == 0), stop=(cch == 1)), reads=K('fs', 1) + K('onesf'), writes=K('ps', 5))
        mean = self.tmpA[:, 0:NT]; tb = self.tmpB[:, 0:NT]
        p.op('dve', lambda e: e.tensor_scalar(out=mean, in0=self.ps[4][:, 0:NT], scalar1=1.0 / 256, scalar2=None, op0=ALU.mult),
             reads=K('ps', 4), writes=K('tmpA'))
        p.op('dve', lambda e: e.tensor_tensor(out=tb, in0=mean, in1=mean, op=ALU.mult), reads=K('tmpA'), writes=K('tmpB'))
        p.op('dve', lambda e: e.scalar_tensor_tensor(out=tb, in0=self.ps[5][:, 0:NT], scalar=1.0 / 256, in1=tb,
                                                     op0=ALU.mult, op1=ALU.subtract), reads=K('ps', 5) + K('tmpB'), writes=K('tmpB'))
        p.op('act', lambda e: e.activation(out=tb, in_=tb, func=AF.Sqrt, bias=self.epsc[:, 0:1], scale=1.0),
             reads=K('tmpB') + K('epsc'), writes=K('tmpB'))
        p.op('dve', lambda e: e.reciprocal(out=tb, in_=tb), reads=K('tmpB'), writes=K('tmpB'))
        for cch in range(2):
            t = self.ntb[cch][:, 0:NT]
            p.op('dve', lambda e, cch=cch, t=t: e.tensor_tensor(out=t, in0=yb[:, cch, 0:NT], in1=mean, op=ALU.subtract),
                 reads=K('fs', 0) + K('tmpA'), writes=K('ntb', cch))
            p.op('dve', lambda e, cch=cch, t=t: e.tensor_tensor(out=t, in0=t, in1=tb, op=ALU.mult),
                 reads=K('ntb', cch) + K('tmpB'), writes=K('ntb', cch))
            p.op('act', lambda e, cch=cch, t=t, tq=tq: e.activation(
                out=self.catT[:, 6 + cch, tq * NT:(tq + 1) * NT], in_=t, func=AF.Silu,
                scale=cs[:, l, cch, 32:33], bias=cs[:, l, cch, 33:34]),
                reads=K('ntb', cch) + K('cfsc'), writes=K('catT', tq))
MK.conformer_unit = _conformer_unit

def _stage5(self, l, u):
    p = self.p
    L = u.L; NT = u.NT; nq = u.nq; b = u.b
    h2b = self.av(0, 8 * NT).rearrange("p (c s) -> p c s", c=8)
    h2tm = [self.av(4096 + i * 1024, 1024) for i in range(2)]
    h2f = self.av(8192, 8 * NT, F32).rearrange("p (c s) -> p c s", c=8); hk = K('h2f')
    wo = []
    for half in range(2):
        wo.append(self.load_w(self.w_out[l, :, half * 512:(half + 1) * 512].rearrange("(kc p) c -> p kc c", p=128)))
    H2 = self.H2S if u.kind == 'lat' else self.H2CS
    AFF = self.AFFS if u.kind == 'lat' else self.AFFCS
    def partA(tq):
        fsb, xk = self.nfs()
        xc = fsb[:, :, 0:NT]
        p.dma('sp', xc, self.xs_ap(u, tq), reads=self.xs_key(u, tq), writes=xk)
        for dc in range(8):
            wb, wk = wo[dc // 4]
            bank = self.nbank()
            for fc in range(8):
                p.op('pe', lambda e, dc=dc, fc=fc, bank=bank, wb=wb, tq=tq: e.matmul(
                    self.ps[bank][:, 0:NT], lhsT=wb[:, fc, (dc % 4) * 128:(dc % 4 + 1) * 128],
                    rhs=self.catT[:, fc, tq * NT:(tq + 1) * NT], start=(fc == 0), stop=(fc == 7)),
                    reads=wk + K('catT', tq), writes=K('ps', bank))
            p.op('dve', lambda e, dc=dc, bank=bank, xc=xc: e.scalar_tensor_tensor(
                out=xc[:, dc, :], in0=self.ps[bank][:, 0:NT], scalar=self.mods[:, 2 * 8 + dc, u.v:u.v + 1],
                in1=xc[:, dc, :], op0=ALU.mult, op1=ALU.add), reads=K('ps', bank) + K('mods') + xk, writes=xk)
        p.dma('sp', self.xs_ap(u, tq), xc, reads=xk, writes=self.xs_key(u, tq))
        return xc, xk
    def partB(tq, xc, xk):
        self.norm_mod(xc, xk, NT, 1, u.v, lambda kc: h2f[:, kc, :], hk)
        for kc in range(8):
            p.op('pe', lambda e, kc=kc: e.matmul(self.ps[4][0:16, 0:NT], lhsT=self.rw[:, l, kc, :], rhs=h2f[:, kc, :],
                                                 start=(kc == 0), stop=(kc == 7)), reads=hk + K('rw'), writes=K('ps', 4))
        ex = self.tmpA[0:16, 0:NT]; rs = self.tmpB[0:16, 0:NT]
        p.op('act', lambda e: e.activation(out=ex, in_=self.ps[4][0:16, 0:NT], func=AF.Exp), reads=K('ps', 4), writes=K('tmpA'))
        p.op('pe', lambda e: e.matmul(self.ps[5][0:16, 0:NT], lhsT=self.onesf[0:16, 0:16], rhs=ex, start=True, stop=True),
             reads=K('tmpA') + K('onesf'), writes=K('ps', 5))
        p.op('dve', lambda e: e.reciprocal(out=rs, in_=self.ps[5][0:16, 0:NT]), reads=K('ps', 5), writes=K('tmpB'))
        p.op('dve', lambda e: e.tensor_tensor(out=ex, in0=ex, in1=rs, op=ALU.mult), reads=K('tmpA') + K('tmpB'), writes=K('tmpA'))
        p.dma('sp', AFF[16 * b:16 * b + 16, tq * NT:(tq + 1) * NT], ex, reads=K('tmpA'), writes=K('AFF' + u.kind))
        p.op('act', lambda e: e.activation(out=h2b[:, 0:4, :], in_=h2f[:, 0:4, :], func=AF.Copy), reads=hk, writes=K('h2b'))
        p.op('dve', lambda e: e.tensor_copy(out=h2b[:, 4:8, :], in_=h2f[:, 4:8, :]), reads=hk, writes=K('h2b'))
        for tt in range(NT // 128):
            for kc in range(8):
                p.op('pe', lambda e, kc=kc, tt=tt: e.transpose(out=self.psb[:, kc * 128:(kc + 1) * 128],
                                                             in_=h2b[:, kc, tt * 128:(tt + 1) * 128], identity=self.identb[:]),
                     reads=K('h2b') + K('identb'), writes=K('psb'))
            hm = h2tm[tt % 2]; hmk = K('h2tm', tt % 2)
            self.evac(hm, self.psb[:, :], K('psb'), hmk)
            p.dma('sp', H2[b, tq * (NT // 128) + tt], hm, reads=hmk, writes=K('H2' + u.kind, b))
    cur = partA(0)
    for tq in range(nq):
        nxt = partA(tq + 1) if tq + 1 < nq else None
        partB(tq, *cur)
        cur = nxt
MK.stage5 = _stage5


def _declare_moe(self):
    nl = self.nl; nb = self.nb
    self.R = 16 * nb
    self.ew1 = self.din("expert_w1", [nl, 16, D, 2 * D])
    self.ew3 = self.din("expert_w3", [nl, 16, D, 2 * D])
    self.ew2 = self.din("expert_w2", [nl, 16, 2 * D, D])
    self.IDXCT = self.dscr("IDXCT", [32, 2, self.R])
    self.MO = [self.dscr("MO%d" % h, [nb * S, 512]) for h in range(2)]
    self.MOC = [self.dscr("MOC%d" % h, [nb * CTX, 512]) for h in range(2)]
    self.out = self.dout("out", [nb, S, D])
    R = self.R
    f0 = self.fs[0].rearrange("p a b -> p (a b)")
    self.GI = f0[:, 0:2 * R].bitcast(U32)
    self.GV = f0[:, 2 * R:4 * R]
    self.GIC = f0[:, 4 * R:4 * R + 16].bitcast(U32)
    self.GVC = f0[:, 4 * R + 16:4 * R + 32]
    self.tmpc = f0[:, 4 * R + 32:4 * R + 64].rearrange("p (w e) -> p w e", w=2)
MK.declare_moe = _declare_moe

def _topk(self, aff_d, L, cap, post):
    p = self.p; R = self.R
    o = 0
    affw = self.bv(o, L, F32)[0:R, :]; o += 2 * L
    vals = self.bv(o, cap, F32)[0:R, :]; o += 2 * cap
    idxu = self.bv(o, cap, U32)[0:R, :]; o += 2 * cap
    idxf = self.bv(o, cap, F32)[0:R, :]; o += 2 * cap
    tT = self.bv(o, 2 * R, F32); o += 4 * R
    p.dma('sp', affw, aff_d, writes=K('affw'))
    for r in range(cap // 8):
        sl = slice(r * 8, (r + 1) * 8)
        p.op('dve', lambda e, sl=sl: e.max(out=vals[:, sl], in_=affw), reads=K('affw'), writes=K('vals'))
        p.op('dve', lambda e, sl=sl: e.max_index(out=idxu[:, sl], in_max=vals[:, sl], in_values=affw),
             reads=K('affw') + K('vals'), writes=K('idxu'))
        p.op('dve', lambda e, sl=sl: e.match_replace(out=affw, in_to_replace=vals[:, sl], in_values=affw, imm_value=-1.0),
             reads=K('affw') + K('vals'), writes=K('affw'))
    p.op('dve', lambda e: e.tensor_copy(out=idxf, in_=idxu), reads=K('idxu'), writes=K('idxf'))
    for cc in range((cap + 127) // 128):
        n = min(128, cap - cc * 128)
        for wi, src in enumerate((idxf, vals)):
            p.op('pe', lambda e, cc=cc, n=n, wi=wi, src=src: e.transpose(
                out=self.ps[wi][0:n, 0:R], in_=src[:, cc * 128:cc * 128 + n], identity=self.identf[0:R, 0:R]),
                reads=K('idxf') + K('vals') + K('cst'), writes=K('ps', wi))
            self.evac(tT[0:n, wi * R:(wi + 1) * R], self.ps[wi][0:n, 0:R], K('ps', wi), K('tT'))
        post(cc, n, tT)
MK.topk = _topk

def _stage6(self, l, do_ctx):
    p = self.p; R = self.R
    def st_lat(cc, n, tT):
        p.op('dve', lambda e: e.tensor_tensor(out=self.GI[:, cc * R:(cc + 1) * R], in0=tT[:, 0:R], in1=self.rowoff[:, 0:R], op=ALU.add),
             reads=K('tT') + K('cst'), writes=K('GI'))
        p.op('dve', lambda e: e.tensor_copy(out=self.GV[:, cc * R:(cc + 1) * R], in_=tT[:, R:2 * R]), reads=K('tT'), writes=K('GV'))
    self.topk(self.AFFS, S, 256, st_lat)
    if do_ctx:
        p.barrier()
        def st_ctx(cc, n, tT):
            p.dma('sp', self.IDXCT, tT[0:32, 0:2 * R].rearrange("p (w r) -> p w r", w=2), reads=K('tT'), writes=K('IDXCT'))
            for b in range(self.nb):
                p.dma('sp', self.tmpc[32 * b:32 * b + 32, :, :], self.IDXCT[:, :, 16 * b:16 * b + 16], reads=K('IDXCT'), writes=K('tmpc'))
            p.op('dve', lambda e: e.tensor_scalar(out=self.GIC[0:32 * self.nb], in0=self.tmpc[0:32 * self.nb, 0, :], scalar1=self.poff[0:32 * self.nb, 0:1],
                                                  scalar2=None, op0=ALU.add), reads=K('tmpc') + K('cst'), writes=K('GIC'))
            p.op('dve', lambda e: e.tensor_copy(out=self.GVC[0:32 * self.nb], in_=self.tmpc[0:32 * self.nb, 1, :]), reads=K('tmpc'), writes=K('GVC'))
        self.topk(self.AFFCS, CTX, 32, st_ctx)
MK.stage6 = _stage6

def _zero_mo(self, do_ctx):
    p = self.p
    z = self.fs[1]
    p.op('dve', lambda e: e.memset(z[:], 0.0), writes=K('fs', 1))
    rows = self.nb * S
    for h in range(2):
        for r0 in range(0, rows, 1024):
            p.dma('sp', self.MO[h][r0:r0 + 1024, :].rearrange("(n p) d -> p n d", p=128), z[:], reads=K('fs', 1), writes=K('MO', h))
        if do_ctx:
            nr = self.nb * CTX
            p.dma('sp', self.MOC[h][0:nr, :].rearrange("(n p) d -> p n d", p=128), z[:, 0:nr // 128, :], reads=K('fs', 1), writes=K('MOC', h))
MK.zero_mo = _zero_mo

def _stage8(self, l, do_ctx):
    p = self.p; nb = self.nb; R = self.R
    nlat = 2 * nb
    nch = nlat + (1 if do_ctx else 0)
    NS = nch * 128
    ncs = 32 * nb
    o = 0
    xeT = self.bv(o, 8 * NS).rearrange("p (k n) -> p k n", k=8); o += 8 * 1152
    xg = self.bv(o, 9 * 1024).rearrange("p (c d) -> p c d", c=9); o += 9 * 1024
    gT = self.bv(o, 16 * NS).rearrange("p (f n) -> p f n", f=16); o += 16 * 1152
    w2h = [self.bv(o + i * 8192, 8192).rearrange("p (f d) -> p f d", f=16) for i in range(2)]; o += 16384
    wbm = [self.bv(o + i * 4096, 4096).rearrange("p (k c) -> p k c", k=8) for i in range(4)]; o += 16384
    assert o <= 32768 + NARENA, o
    ysc = self.sqb
    splits = [(n0, min(512, NS - n0)) for n0 in range(0, NS, 512)] if NS % 384 else [(n0, 384) for n0 in range(0, NS, 384)]
    H2f = self.H2S.rearrange("b t p d -> (b t p) d")
    H2cf = self.H2CS.rearrange("b t p d -> (b t p) d")
    if do_ctx and ncs < 128:
        p.op('dve', lambda e: e.memset(xg[:, nlat, :], 0.0), writes=K('xg', nlat))
    wi = 0
    def gathers(e_):
        for ch in range(nch):
            if ch < nlat:
                b, cc = ch // 2, ch % 2
                col = self.GI[:, cc * R + 16 * b + e_:cc * R + 16 * b + e_ + 1]
                p.dma('pool', None, None, fn=lambda e, ch=ch, col=col: e.indirect_dma_start(
                    xg[:, ch, :], None, H2f, bass.IndirectOffsetOnAxis(ap=col, axis=0)), reads=K('GI'), writes=K('xg', ch))
            else:
                col = self.GIC[0:ncs, e_:e_ + 1]
                p.dma('pool', None, None, fn=lambda e, ch=ch, col=col: e.indirect_dma_start(
                    xg[0:ncs, ch, :], None, H2cf, bass.IndirectOffsetOnAxis(ap=col, axis=0)), reads=K('GIC'), writes=K('xg', ch))
    def w13(e_, Fg):
        nonlocal wi
        wa = wbm[wi % 4]; wak = K('wbm', wi % 4); wi += 1
        wu = wbm[wi % 4]; wuk = K('wbm', wi % 4); wi += 1
        p.dma('pool', wa, self.ew1[l, e_, :, Fg * 512:(Fg + 1) * 512].rearrange("(k p) c -> p k c", p=128), writes=wak)
        p.dma('pool', wu, self.ew3[l, e_, :, Fg * 512:(Fg + 1) * 512].rearrange("(k p) c -> p k c", p=128), writes=wuk)
        return wa, wak, wu, wuk
    def w2load(e_, dh):
        p.dma('pool', w2h[dh], self.ew2[l, e_, :, dh * 512:(dh + 1) * 512].rearrange("(f p) d -> p f d", p=128), writes=K('w2h', dh))
    gathers(0)
    pre13 = {0: [w13(0, 0), w13(0, 1)]}
    w2load(0, 0); w2load(0, 1)
    for e_ in range(16):
        for ch in range(nch):
            for kc in range(8):
                p.op('pe', lambda e, ch=ch, kc=kc: e.transpose(out=self.psb[:, kc * 128:(kc + 1) * 128],
                                                             in_=xg[:, ch, kc * 128:(kc + 1) * 128], identity=self.identb[:]),
                     reads=K('xg', ch) + K('identb'), writes=K('psb'))
            self.evac(xeT[:, :, ch * 128:(ch + 1) * 128], self.psb[:, :].rearrange("p (k s) -> p k s", k=8), K('psb'), K('xeT'))
        for Fg in range(4):
            if Fg < 2:
                wa, wak, wu, wuk = pre13[e_][Fg]
            else:
                wa, wak, wu, wuk = w13(e_, Fg)
            for fl in range(4):
                fcn = Fg * 4 + fl
                for si, (n0, nn) in enumerate(splits):
                    bA = self.nbank(); bU = self.nbank()
                    for kc in range(8):
                        p.op('pe', lambda e, kc=kc, fl=fl, n0=n0, nn=nn, bA=bA, wa=wa: e.matmul(
                            self.ps[bA][:, 0:nn], lhsT=wa[:, kc, fl * 128:(fl + 1) * 128], rhs=xeT[:, kc, n0:n0 + nn],
                            start=(kc == 0), stop=(kc == 7)), reads=wak + K('xeT'), writes=K('ps', bA))
                    for kc in range(8):
                        p.op('pe', lambda e, kc=kc, fl=fl, n0=n0, nn=nn, bU=bU, wu=wu: e.matmul(
                            self.ps[bU][:, 0:nn], lhsT=wu[:, kc, fl * 128:(fl + 1) * 128], rhs=xeT[:, kc, n0:n0 + nn],
                            start=(kc == 0), stop=(kc == 7)), reads=wuk + K('xeT'), writes=K('ps', bU))
                    sg = self.ntb[si % 2][:, 0:nn]; sgk = K('ntb', si % 2)
                    p.op('act', lambda e, sg=sg, bA=bA, nn=nn: e.activation(out=sg, in_=self.ps[bA][:, 0:nn], func=AF.Silu),
                         reads=K('ps', bA), writes=sgk)
                    p.op('dve', lambda e, sg=sg, bU=bU, nn=nn, fcn=fcn, n0=n0: e.tensor_tensor(
                        out=gT[:, fcn, n0:n0 + nn], in0=self.ps[bU][:, 0:nn], in1=sg, op=ALU.mult),
                        reads=K('ps', bU) + sgk, writes=K('gT'))
        if e_ + 1 < 16:
            gathers(e_ + 1)
            pre13[e_ + 1] = [w13(e_ + 1, 0), w13(e_ + 1, 1)]
        yi = 0
        for dh in range(2):
            for ch in range(nch):
                bank = self.nbank()
                for fc in range(16):
                    p.op('pe', lambda e, fc=fc, ch=ch, dh=dh, bank=bank: e.matmul(
                        self.ps[bank], lhsT=gT[:, fc, ch * 128:(ch + 1) * 128], rhs=w2h[dh][:, fc, :],
                        start=(fc == 0), stop=(fc == 15)), reads=K('gT') + K('w2h', dh), writes=K('ps', bank))
                ys = ysc[yi % 2]; ysk = K('sqb', yi % 2); yi += 1
                if ch < nlat:
                    b, cc = ch // 2, ch % 2
                    gcol = self.GV[:, cc * R + 16 * b + e_:cc * R + 16 * b + e_ + 1]; gk = K('GV')
                    icol = self.GI[:, cc * R + 16 * b + e_:cc * R + 16 * b + e_ + 1]; ik = K('GI')
                    dst = self.MO[dh]; dk = K('MO', dh, b); npart = 128
                else:
                    gcol = self.GVC[0:ncs, e_:e_ + 1]; gk = K('GVC')
                    icol = self.GIC[0:ncs, e_:e_ + 1]; ik = K('GIC')
                    dst = self.MOC[dh]; dk = K('MOC', dh); npart = ncs
                eng = 'act' if yi % 2 else 'dve'
                if eng == 'act':
                    p.op('act', lambda e, ys=ys, bank=bank, gcol=gcol, npart=npart: e.activation(
                        out=ys[0:npart], in_=self.ps[bank][0:npart], func=AF.Identity, scale=gcol), reads=K('ps', bank) + gk, writes=ysk)
                else:
                    p.op('dve', lambda e, ys=ys, bank=bank, gcol=gcol, npart=npart: e.tensor_scalar(
                        out=ys[0:npart], in0=self.ps[bank][0:npart], scalar1=gcol, scalar2=None, op0=ALU.mult),
                        reads=K('ps', bank) + gk, writes=ysk)
                p.dma('pool', None, None, fn=lambda e, ys=ys, dst=dst, icol=icol, npart=npart: e.indirect_dma_start(
                    dst, bass.IndirectOffsetOnAxis(ap=icol, axis=0), ys[0:npart], None, compute_op=ALU.add),
                    reads=ysk + ik, writes=dk)
            if e_ + 1 < 16:
                w2load(e_ + 1, dh)
MK.stage8 = _stage8

def _scatter_setup(self, u):
    p = self.p; b = u.b; NT = u.NT
    MOs = self.MO if u.kind == 'lat' else self.MOC
    row0 = b * u.L
    def pre(tq, xc, xk):
        for tt in range(NT // 128):
            tok0 = row0 + tq * NT + tt * 128
            xt = self.xtm[:, tt % 2, :]; xtk = K('xtm', tt % 2)
            for h in range(2):
                p.dma('sp', xt[:, h * 512:(h + 1) * 512], MOs[h][tok0:tok0 + 128, :], reads=K('MO' + u.kind), writes=xtk)
            for half in range(2):
                bank = self.nbank()
                for j in range(4):
                    kc = half * 4 + j
                    p.op('pe', lambda e, kc=kc, j=j, bank=bank, xt=xt: e.transpose(
                        out=self.ps[bank][:, j * 128:(j + 1) * 128], in_=xt[:, kc * 128:(kc + 1) * 128],
                        identity=self.identf), reads=xtk + K('cst'), writes=K('ps', bank))
                for j in range(4):
                    kc = half * 4 + j
                    p.op('dve', lambda e, kc=kc, j=j, bank=bank, tt=tt: e.scalar_tensor_tensor(
                        out=xc[:, kc, tt * 128:(tt + 1) * 128], in0=self.ps[bank][:, j * 128:(j + 1) * 128],
                        scalar=self.g2p[:, kc, u.v:u.v + 1], in1=xc[:, kc, tt * 128:(tt + 1) * 128], op0=ALU.mult, op1=ALU.add),
                        reads=K('ps', bank) + K('g2p') + xk, writes=xk)
    return pre
MK.scatter_setup = _scatter_setup

def _final_unit(self, u, pre):
    p = self.p; NT = u.NT
    otm = [self.bv(0 + i * 2048, 1024, F32) for i in range(2)]
    of = self.bv(4096, 8 * NT, F32).rearrange("p (c s) -> p c s", c=8); ok = K('of')
    oi = 0
    def fetch(tq):
        fsb, xk = self.nfs()
        xc = fsb[:, :, 0:NT]
        p.dma('sp', xc, self.xs_ap(u, tq), reads=self.xs_key(u, tq), writes=xk)
        pre(tq, xc, xk)
        return xc, xk
    cur = fetch(0)
    for tq in range(u.nq):
        nxt = fetch(tq + 1) if tq + 1 < u.nq else None
        xc, xk = cur
        self.norm_mod(xc, xk, NT, 2, u.v, lambda kc: of[:, kc, :], ok)
        for tt in range(NT // 128):
            ot = otm[oi % 2]; otk = K('otm', oi % 2); oi += 1
            for half in range(2):
                bank = self.nbank()
                for j in range(4):
                    kc = half * 4 + j
                    p.op('pe', lambda e, kc=kc, j=j, tt=tt, bank=bank: e.transpose(
                        out=self.ps[bank][:, j * 128:(j + 1) * 128], in_=of[:, kc, tt * 128:(tt + 1) * 128],
                        identity=self.identf), reads=ok + K('cst'), writes=K('ps', bank))
                self.evac(ot[:, half * 512:(half + 1) * 512], self.ps[bank], K('ps', bank), otk)
            tok0 = tq * NT + tt * 128
            p.dma('sp', self.out[u.b, tok0:tok0 + 128, :], ot, reads=otk, writes=K('out', u.b, tq, tt))
        cur = nxt
MK.final_unit = _final_unit

def _build(self, sched):
    p = self.p; nb = self.nb; nl = self.nl
    self.declare(); self.declare_hyena(); self.declare_cf(); self.declare_moe()
    if getattr(self, 'dbg', None): self.dbg('decl', 0, None)
    self.init_consts()
    lat = [Unit('lat', b, nb) for b in range(nb)]
    ctxu = [Unit('ctx', b, nb) for b in range(nb)]
    for l in range(nl):
        last = (l == nl - 1)
        p.barrier()
        if l > 0:
            p.op('dve', lambda e: e.tensor_copy(out=self.g2p[:], in_=self.mods[:, 40:48, :]), reads=K('mods'), writes=K('g2p'))
        p.stage = 'adaln'
        self.adaln(l)
        p.stage = 'filter'
        self.hyena_filter(l, S); p.barrier()
        if not last:
            self.hyena_filter(l, CTX); p.barrier()
        for b in range(nb):
            for u in (ctxu[b], lat[b]):
                pre = None
                if l > 0:
                    p.stage = 'scatter'
                    pre = self.scatter_setup(u)
                if pre is None: p.stage = 'stage1'
                self.stage1(l, u, pre); p.barrier(True)
                p.stage = 'attn'
                if getattr(self, 'dbg', None): self.dbg('s1', l, u)
                self.attn_stage(l, u, sched, last); p.barrier(True)
                if u.kind == 'ctx' and last:
                    continue
                p.stage = 'hyena'
                self.hyena_unit(l, u); p.barrier(True)
                p.stage = 'conformer'
                self.conformer_unit(l, u); p.barrier(True)
                p.stage = 'stage5'
                self.stage5(l, u); p.barrier(True)
                if getattr(self, 'dbg', None): self.dbg('s5', l, u)
        p.stage = 'topk'
        p.barrier()
        self.zero_mo(do_ctx=not last)
        self.stage6(l, do_ctx=not last); p.barrier()
        p.stage = 'experts'
        self.stage8(l, do_ctx=not last); p.barrier()
    p.op('dve', lambda e: e.tensor_copy(out=self.g2p[:], in_=self.mods[:, 40:48, :]), reads=K('mods'), writes=K('g2p'))
    p.stage = 'final'
    for b in range(nb):
        pre = self.scatter_setup(lat[b])
        self.final_unit(lat[b], pre); p.barrier()
    p.finish()
MK.build = _build


GRID_W = 64; ROWS = 32; WIN_H = 8; WIN_W = 16

def na_schedule():
    r0 = lambda r: min(max(r - WIN_H // 2, 0), ROWS - WIN_H)
    specs = {}; sched = []
    for i in range(16):
        lst = []
        for j in range(16):
            dr = [[(2 * j + a) - (2 * i + b) for b in range(2)] for a in range(2)]
            iw = [[int(r0(2 * i + b) <= 2 * j + a < r0(2 * i + b) + WIN_H) for b in range(2)] for a in range(2)]
            if not any(iw[a][b] for a in range(2) for b in range(2)):
                continue
            key = (tuple(map(tuple, dr)), tuple(map(tuple, iw)))
            if key not in specs:
                specs[key] = len(specs)
            lst.append((j, specs[key]))
        sched.append(lst)
    keys = list(specs.keys())
    order = [t for (_, t) in sched[7]]
    order += [t for t in range(len(keys)) if t not in order]
    remap = {old: new for new, old in enumerate(order)}
    sched = [[(j, remap[t]) for (j, t) in lst] for lst in sched]
    return sched, [keys[old] for old in order]

def na_bias_table(rpb):
    sched, specs = na_schedule()
    H = rpb.shape[0]; NT = len(specs)
    col = np.arange(GRID_W)
    c0 = np.clip(col - WIN_W // 2, 0, GRID_W - WIN_W)
    col_ok = (col[None, :] >= c0[:, None]) & (col[None, :] < c0[:, None] + WIN_W)
    dc = np.clip(col[None, :] - col[:, None], 1 - WIN_W, WIN_W - 1) + WIN_W - 1
    tab = np.full((2, 64, H, NT, 2, 64), -1e30, dtype=np.float32)
    for t, (dr, iw) in enumerate(specs):
        for a in range(2):
            for b in range(2):
                if not iw[a][b]:
                    continue
                dri = dr[a][b] + WIN_H - 1
                vals = rpb[:, dri, :][:, dc]
                vals = np.where(col_ok[None], vals, np.float32(-1e30))
                tab[a, :, :, t, b, :] = vals.transpose(2, 0, 1)
    return np.ascontiguousarray(tab.reshape(128, H, NT, 128))

def base_inputs(inp, b0, nb):
    c = inp["c"][b0:b0 + nb]; c_ctx = inp["c_ctx"]
    cv = np.concatenate([c, c_ctx[None]], 0)
    cT = np.ascontiguousarray(cv.reshape(nb + 1, 8, 128).transpose(2, 1, 0))
    nl = inp["w_mod"].shape[0]
    b_modT = np.ascontiguousarray(inp["b_mod"].reshape(nl, 48, 128).transpose(2, 0, 1))
    ng = np.stack([inp["norm1_g"][l] for l in range(nl)] + [inp["norm2_g"][l] for l in range(nl)] + [inp["final_norm_g"]], 0)
    ngT = np.ascontiguousarray(ng.reshape(2 * nl + 1, 8, 128).transpose(2, 0, 1))
    cst = np.zeros((128, 128 + 512 + 16 + 64 + 1), np.float32)
    cst[:, 0:128] = np.eye(128)
    cst[:, 128:640] = np.arange(512)[None, :]
    cst[:, 640:656] = np.arange(128)[:, None] + 128 * np.arange(16)[None, :]
    cst[:, 656:720] = 2048 * (np.arange(64) // 16)[None, :]
    cst[:, 720] = 256 * (np.arange(128) // 32)
    m = {
        "x": np.ascontiguousarray(inp["x"][b0:b0 + nb]),
        "ctx": np.ascontiguousarray(inp["ctx"][b0:b0 + nb]),
        "cT": cT, "w_mod": inp["w_mod"], "b_modT": b_modT, "ngT": ngT, "w_in": inp["w_in"], "w_out": inp["w_out"],
        "cst_f": cst, "ident_b": np.eye(128).astype(BF),
        "na_tab": np.stack([na_bias_table(inp["na_rpb"][l]) for l in range(nl)]),
    }
    return m

import math
def dft_consts(L):
    N = 2 * L
    nC = L // 128; NT = min(512, L); nq = L // NT; FG = min(4, nC); nfg = nC // FG
    t = np.arange(L, dtype=np.float64); f = np.arange(L, dtype=np.float64) + 0.5
    ang = 2 * np.pi * np.outer(t, f) / N
    C = np.cos(ang); Sn = np.sin(ang)
    A = np.stack([C, Sn], 0).reshape(2, nC, 128, nC, 128)
    A = np.ascontiguousarray(A.transpose(3, 2, 0, 1, 4)).astype(BF)
    Bm = np.stack([C.T, Sn.T], 0).reshape(2, nfg, FG, 128, nq, NT)
    Bm = np.ascontiguousarray(Bm.transpose(4, 1, 3, 0, 2, 5)).astype(BF)
    return A, Bm

def hyena_pos_consts(L):
    f32 = np.float32
    pos = np.arange(L, dtype=f32)[:, None]
    t = pos / f32(max(L - 1, 1))
    bands = 16
    fb = np.linspace(1e-4, bands - 1, bands, dtype=f32)[None, :]
    ang = fb * f32(2.0 * math.pi) * pos / f32(L)
    z = np.concatenate([t, np.cos(ang), -np.sin(ang)], axis=-1).astype(f32)
    zT = np.zeros((64, L), f32); zT[:33] = z.T
    min_decay = math.log(1e-2) / 1.5; max_decay = math.log(1e-2) / 0.3
    rate = np.abs(np.linspace(min_decay, max_decay, 256, dtype=f32))
    dec = np.exp(-t * rate).astype(f32)
    decb = dec.copy(); decb[0] = 0.0
    dd = np.stack([dec, decb], 1).reshape(L // 128, 128, 2, 256).transpose(1, 0, 2, 3)
    return zT, np.ascontiguousarray(dd)

def hyena_inputs(inp):
    nl = inp["hy_filt_w1"].shape[0]
    m = {}
    for L in (2048, 256):
        A, Bm = dft_consts(L)
        zT, dd = hyena_pos_consts(L)
        m["dftA%d" % L] = A; m["dftB%d" % L] = Bm; m["zpos%d" % L] = zT; m["decay%d" % L] = dd
    hyw = np.zeros((nl, 64, 64 + 64 + 1024 + 4), np.float32)
    hyw[:, :33, 0:64] = inp["hy_filt_w1"]
    hyw[:, :, 64:128] = inp["hy_filt_w2"]
    hyw[:, :, 128:1152] = inp["hy_filt_w3"]
    hyw[:, :, 1152] = inp["hy_filt_b1"]; hyw[:, :, 1153] = inp["hy_filt_freq"]; hyw[:, :, 1154] = inp["hy_filt_b2"]
    m["hyw"] = hyw
    sw = inp["hy_short_w"].reshape(nl, 3, 6, 128).transpose(0, 3, 2, 1)
    sb = inp["hy_short_b"].reshape(nl, 6, 128).transpose(0, 2, 1)[..., None]
    dd = inp["hy_bias_d"].reshape(nl, 2, 2, 128).transpose(0, 3, 1, 2)
    m["hysc"] = np.ascontiguousarray(np.concatenate([sw.reshape(nl, 128, 18), sb.reshape(nl, 128, 6), dd.reshape(nl, 128, 4)], -1).transpose(1, 0, 2))
    return m

def cf_moe_inputs(inp):
    nl = inp["cf_dw_w"].shape[0]
    m = {}
    cw = inp["cf_dw_w"].reshape(nl, 31, 2, 128).transpose(3, 0, 2, 1)
    oth = np.stack([inp["cf_dw_b"], inp["cf_ln_g"], inp["cf_ln_b"]], 1).reshape(nl, 3, 2, 128).transpose(3, 0, 2, 1)
    m["cfsc"] = np.ascontiguousarray(np.concatenate([cw, oth], -1))
    m["router_wT"] = np.ascontiguousarray(inp["router_w"].reshape(nl, 8, 128, 16).transpose(2, 0, 1, 3))
    m["expert_w1"] = inp["expert_w1"]; m["expert_w3"] = inp["expert_w3"]; m["expert_w2"] = inp["expert_w2"]
    return m


NCORES = 8

def kernel(**inputs):
    from concourse.bass_utils import run_bass_kernel_spmd
    inp = {k: np.asarray(v) for k, v in inputs.items()}
    B = inp["x"].shape[0]
    nb = B // NCORES
    sched, _ = na_schedule()
    mk = MK(nb=nb, nl=NL)
    mk.build(sched)
    shared = {}
    shared.update(hyena_inputs(inp))
    shared.update(cf_moe_inputs(inp))
    in_maps = []
    for c in range(NCORES):
        m = base_inputs(inp, c * nb, nb)
        m.update(shared)
        in_maps.append({k: v for k, v in m.items() if k in mk.dram})
    res = run_bass_kernel_spmd(mk.nc, in_maps, core_ids=list(range(NCORES)))
    out = np.concatenate([np.asarray(r["out"]) for r in res.results], axis=0)
    return out.astype(np.float32, copy=False)
```
